# Optimizing a Trainium2 kernel written in Bass

```python
import math
import jax
import jax.numpy as jnp
from jax import lax
import numpy as np

D_MODEL = 1024
BATCH = 16
SEQ = 2048
DEPTH = 2

GRID_W = 64
CTX_LEN = 256
NA_HEADS = 8
HEAD_DIM = 64
NA_WIDTH = NA_HEADS * HEAD_DIM
WIN_H_MAX = 8
WIN_W = 16
QBLK_W = 16
KBLK_W = QBLK_W + WIN_W
ROPE_THETA = 100.0
HY_WIDTH = 512
HY_ORDER = 2
HY_SHORT = 3
HY_BANDS = 16
HY_EMB = 1 + 2 * HY_BANDS
HY_FILTER_HIDDEN = 64
HY_SIN_FREQ = 1.0
HY_MAX_DECAY = math.log(1e-2) / 0.3
HY_MIN_DECAY = math.log(1e-2) / 1.5
N_BRANCH = 2
IN_COLS = 3 * NA_WIDTH + (HY_ORDER + 1) * HY_WIDTH + N_BRANCH * D_MODEL
N_GROUPS = 4
EXPERTS_PER_GROUP = 8
N_EXPERTS = N_GROUPS * EXPERTS_PER_GROUP
TOP_K = 2
D_EXPERT = 256
EPS = 1e-6
F32 = jnp.float32

kernel_name = 'hybrid_natten_hyena_hmoe_block'


def _rms(x, g):
    x32 = x.astype(F32)
    y = x32 * lax.rsqrt(jnp.mean(x32 * x32, axis=-1, keepdims=True) + EPS)
    return (y * g.astype(F32)).astype(x.dtype)


def _rope_2d(x, rows, cols):
    quarter = x.shape[-1] // 4
    freqs = ROPE_THETA ** (-jnp.arange(quarter, dtype=F32) / quarter)

    def rot(xh, pos):
        ang = pos.astype(F32)[:, None] * freqs[None, :]
        cos = jnp.cos(ang)[None, :, None, :]
        sin = jnp.sin(ang)[None, :, None, :]
        a, b = jnp.split(xh.astype(F32), 2, axis=-1)
        return jnp.concatenate([a * cos - b * sin, b * cos + a * sin], axis=-1)

    xr, xcol = jnp.split(x, 2, axis=-1)
    return jnp.concatenate([rot(xr, rows), rot(xcol, cols)], axis=-1).astype(x.dtype)


def _heads(t):
    return t.reshape(t.shape[0], t.shape[1], NA_HEADS, HEAD_DIM)


def _project(h, w_in, q_norm, k_norm):
    p = h @ w_in
    q, k, v, hy, gates = jnp.split(
        p, [NA_WIDTH, 2 * NA_WIDTH, 3 * NA_WIDTH, 3 * NA_WIDTH + (HY_ORDER + 1) * HY_WIDTH], axis=-1)
    return _rms(_heads(q), q_norm), _rms(_heads(k), k_norm), _heads(v), hy, gates


def _neighbourhood_attention(q_rot, k_rot, v, q_plain, k_ctx, v_ctx, rpb):
    B, L, H, dh = q_rot.shape
    R = L // GRID_W
    kh = min(WIN_H_MAX, R)
    nj = GRID_W // QBLK_W
    scale = dh ** -0.5
    rr = jnp.arange(R)
    row_idx = jnp.clip(rr - kh // 2, 0, R - kh)[:, None] + jnp.arange(kh)[None, :]
    jj = jnp.arange(nj)
    col_idx = jnp.clip(jj * QBLK_W - WIN_W // 2, 0, GRID_W - KBLK_W)[:, None] + jnp.arange(KBLK_W)[None, :]
    qcol = jj[:, None] * QBLK_W + jnp.arange(QBLK_W)[None, :]
    wstart = jnp.clip(qcol - WIN_W // 2, 0, GRID_W - WIN_W)
    kcol = col_idx[:, None, :]
    in_win = (kcol >= wstart[..., None]) & (kcol < wstart[..., None] + WIN_W)
    dr = row_idx - rr[:, None]
    dc = jnp.clip(kcol - qcol[..., None], -(WIN_W - 1), WIN_W - 1)
    bias = rpb[:, dr[:, None, None, :, None] + (WIN_H_MAX - 1), dc[None, :, :, None, :] + (WIN_W - 1)]
    bias = jnp.where(in_win[None, None, :, :, None, :], bias.astype(F32), -jnp.inf)

    def band(t):
        g = t.reshape(B, R, GRID_W, H, dh)[:, row_idx]
        return jnp.take(g, col_idx, axis=3)

    kb, vb = band(k_rot), band(v)
    qg = q_rot.reshape(B, R, nj, QBLK_W, H, dh)
    qp = q_plain.reshape(B, R, nj, QBLK_W, H, dh)
    s_win = jnp.einsum('brjqhd,brajkhd->bhrjqak', qg, kb, preferred_element_type=F32) * scale + bias[None]
    s_ctx = jnp.einsum('brjqhd,bchd->bhrjqc', qp, k_ctx, preferred_element_type=F32) * scale
    n_win = kh * KBLK_W
    s = jnp.concatenate([s_win.reshape(B, H, R, nj, QBLK_W, n_win), s_ctx], axis=-1)
    p = jax.nn.softmax(s, axis=-1)
    p_win = p[..., :n_win].reshape(B, H, R, nj, QBLK_W, kh, KBLK_W).astype(v.dtype)
    p_ctx = p[..., n_win:].astype(v.dtype)
    o = (jnp.einsum('bhrjqak,brajkhd->brjqhd', p_win, vb)
         + jnp.einsum('bhrjqc,bchd->brjqhd', p_ctx, v_ctx))
    return o.reshape(B, L, H * dh)


def _ctx_attention(q, k, v):
    B, L, H, dh = q.shape
    s = jnp.einsum('bqhd,bkhd->bhqk', q, k, preferred_element_type=F32) * (dh ** -0.5)
    p = jax.nn.softmax(s, axis=-1).astype(v.dtype)
    return jnp.einsum('bhqk,bkhd->bqhd', p, v).reshape(B, L, H * dh)


def _hyena_filter_fft(L, w1, b1, w2, b2, w3):
    pos = jnp.arange(L, dtype=F32)
    t = pos / max(L - 1, 1)
    w = 2.0 * math.pi * pos / L
    f = jnp.linspace(1e-4, HY_BANDS - 1, HY_BANDS, dtype=F32)
    z = jnp.concatenate([t[:, None], jnp.cos(f[None, :] * w[:, None]), -jnp.sin(f[None, :] * w[:, None])], axis=-1)
    hid = jnp.sin(HY_SIN_FREQ * (z @ w1 + b1))
    hid = jnp.sin(HY_SIN_FREQ * (hid @ w2 + b2))
    h = (hid @ w3).astype(F32).reshape(L, 2, HY_ORDER, HY_WIDTH)
    deltas = jnp.abs(jnp.linspace(HY_MIN_DECAY, HY_MAX_DECAY, HY_WIDTH, dtype=F32))
    h = h * jnp.exp(-t[:, None] * deltas[None, :])[:, None, None, :]
    h_fwd = h[:, 0]
    h_bwd = h[1:, 1]
    norm = jnp.sum(jnp.abs(h_fwd), axis=0) + jnp.sum(jnp.abs(h_bwd), axis=0) + EPS
    k2 = jnp.concatenate([h_fwd, jnp.zeros((1, HY_ORDER, HY_WIDTH), F32), h_bwd[::-1]], axis=0) / norm
    return jnp.fft.rfft(k2, axis=0)


def _short_conv(u, w, b):
    L = u.shape[1]
    pad = HY_SHORT // 2
    up = jnp.pad(u, ((0, 0), (pad, pad), (0, 0)))
    return sum(up[:, i:i + L] * w[i] for i in range(HY_SHORT)) + b


def _hyena(u, short_w, short_b, filt_fft, skip):
    L = u.shape[1]
    u = _short_conv(u, short_w, short_b)
    v, x1, x2 = jnp.split(u, 3, axis=-1)
    z = v.astype(F32)
    for o, gate in enumerate((x1, x2)):
        zf = jnp.fft.rfft(z, n=2 * L, axis=1)
        conv = jnp.fft.irfft(zf * filt_fft[None, :, o, :], n=2 * L, axis=1)[:, :L]
        z = gate.astype(F32) * (conv + z * skip[o].astype(F32))
    return z.astype(u.dtype)


def _merge(attn, hyena, gates, w_br_a, w_br_b, w_out):
    ga, gb = jnp.split(gates, 2, axis=-1)
    y = jax.nn.sigmoid(ga) * (attn @ w_br_a) + jax.nn.sigmoid(gb) * (hyena @ w_br_b)
    return y @ w_out


def _hier_moe(h, w_group, b_group, w_router, b_router, w1, w3, w2):
    T = h.shape[0]
    glog = (h @ w_group + b_group).astype(F32)
    gprob = jax.nn.softmax(glog, axis=-1)
    gidx = jnp.argmax(glog, axis=-1)
    gp = jnp.take_along_axis(gprob, gidx[:, None], axis=1)[:, 0]
    elog = (h @ w_router + b_router).astype(F32).reshape(T, N_GROUPS, EXPERTS_PER_GROUP)
    elog = jnp.take_along_axis(elog, gidx[:, None, None], axis=1)[:, 0]
    top_v, top_i = lax.top_k(elog, TOP_K)
    wts = gp[:, None] * jax.nn.softmax(top_v, axis=-1)
    eid = gidx[:, None] * EXPERTS_PER_GROUP + top_i
    cw = jnp.einsum('tk,tke->te', wts, jax.nn.one_hot(eid, N_EXPERTS, dtype=F32))
    cw = cw.reshape(T, N_GROUPS, EXPERTS_PER_GROUP).astype(h.dtype)
    y = jnp.zeros(h.shape, F32)
    for g in range(N_GROUPS):
        a = jnp.einsum('td,edf->tef', h, w1[g])
        b = jnp.einsum('td,edf->tef', h, w3[g])
        hid = jax.nn.silu(a) * b * cw[:, g, :, None]
        y = y + jnp.einsum('tef,efd->td', hid, w2[g], preferred_element_type=F32)
    return y.astype(h.dtype)


def _layer(x, xc, c, c_ctx, last, ada_w, ada_b, norm_mix, norm_ffn, w_in, q_norm, k_norm, rpb,
           short_w, short_b, flt_w1, flt_b1, flt_w2, flt_b2, flt_w3, hy_skip, w_br_a, w_br_b, w_out,
           w_group, b_group, w_router, b_router, moe_w1, moe_w3, moe_w2):
    B, L, D = x.shape
    mod = jax.nn.silu(c) @ ada_w + ada_b
    modc = jax.nn.silu(c_ctx) @ ada_w + ada_b
    sh1, sc1, g1, sh2, sc2, g2 = [m[:, None, :] for m in jnp.split(mod, 6, axis=-1)]
    sh1c, sc1c, g1c, sh2c, sc2c, g2c = jnp.split(modc, 6, axis=-1)

    h = _rms(x, norm_mix) * (1.0 + sc1) + sh1
    hc = _rms(xc, norm_mix) * (1.0 + sc1c) + sh1c
    q, k, v, hy, gates = _project(h, w_in, q_norm, k_norm)
    if last:
        kc_, vc_ = jnp.split(hc @ w_in[:, NA_WIDTH:3 * NA_WIDTH], 2, axis=-1)
        kc, vc = _rms(_heads(kc_), k_norm), _heads(vc_)
    else:
        qc, kc, vc, hyc, gatesc = _project(hc, w_in, q_norm, k_norm)
    pos = jnp.arange(L)
    rows, cols = pos // GRID_W, pos % GRID_W
    attn = _neighbourhood_attention(_rope_2d(q, rows, cols), _rope_2d(k, rows, cols), v, q, kc, vc, rpb)
    hyena = _hyena(hy, short_w, short_b, _hyena_filter_fft(L, flt_w1, flt_b1, flt_w2, flt_b2, flt_w3), hy_skip)
    x = x + g1 * _merge(attn, hyena, gates, w_br_a, w_br_b, w_out)

    h2 = _rms(x, norm_ffn) * (1.0 + sc2) + sh2
    x = x + g2 * _hier_moe(h2.reshape(B * L, D), w_group, b_group, w_router, b_router,
                           moe_w1, moe_w3, moe_w2).reshape(B, L, D)
    if last:
        return x, xc

    Lc = xc.shape[1]
    attn_c = _ctx_attention(qc, kc, vc)
    hyena_c = _hyena(hyc, short_w, short_b, _hyena_filter_fft(Lc, flt_w1, flt_b1, flt_w2, flt_b2, flt_w3), hy_skip)
    xc = xc + g1c * _merge(attn_c, hyena_c, gatesc, w_br_a, w_br_b, w_out)
    h2c = _rms(xc, norm_ffn) * (1.0 + sc2c) + sh2c
    xc = xc + g2c * _hier_moe(h2c.reshape(B * Lc, D), w_group, b_group, w_router, b_router,
                              moe_w1, moe_w3, moe_w2).reshape(B, Lc, D)
    return x, xc


def setup_inputs(seed: int = 0) -> dict:
    key = jax.random.key(seed)
    ks = list(jax.random.split(key, 40))
    cnt = [0]

    def nrm(shape, scale):
        k = ks[cnt[0]]
        cnt[0] += 1
        return scale * jax.random.normal(k, shape, F32)

    D = D_MODEL
    G, E, F = N_GROUPS, EXPERTS_PER_GROUP, D_EXPERT
    return {
        'x': nrm((BATCH, SEQ, D), 1.0),
        'c': nrm((BATCH, D), 1.0),
        'ctx': nrm((BATCH, CTX_LEN, D), 1.0),
        'c_ctx': nrm((D,), 1.0),
        'ada_w': nrm((DEPTH, D, 6 * D), 0.5 * D ** -0.5),
        'ada_b': nrm((DEPTH, 6 * D), 0.02),
        'norm_mix': 1.0 + nrm((DEPTH, D), 0.05),
        'norm_ffn': 1.0 + nrm((DEPTH, D), 0.05),
        'w_in': nrm((DEPTH, D, IN_COLS), D ** -0.5),
        'q_norm': 1.0 + nrm((DEPTH, HEAD_DIM), 0.05),
        'k_norm': 1.0 + nrm((DEPTH, HEAD_DIM), 0.05),
        'rpb': nrm((DEPTH, NA_HEADS, 2 * WIN_H_MAX - 1, 2 * WIN_W - 1), 0.1),
        'short_w': nrm((DEPTH, HY_SHORT, (HY_ORDER + 1) * HY_WIDTH), 0.5),
        'short_b': nrm((DEPTH, (HY_ORDER + 1) * HY_WIDTH), 0.02),
        'flt_w1': nrm((DEPTH, HY_EMB, HY_FILTER_HIDDEN), HY_EMB ** -0.5),
        'flt_b1': nrm((DEPTH, HY_FILTER_HIDDEN), 0.1),
        'flt_w2': nrm((DEPTH, HY_FILTER_HIDDEN, HY_FILTER_HIDDEN), HY_FILTER_HIDDEN ** -0.5),
        'flt_b2': nrm((DEPTH, HY_FILTER_HIDDEN), 0.1),
        'flt_w3': nrm((DEPTH, HY_FILTER_HIDDEN, 2 * HY_ORDER * HY_WIDTH), HY_FILTER_HIDDEN ** -0.5),
        'hy_skip': nrm((DEPTH, HY_ORDER, HY_WIDTH), 1.0),
        'w_br_a': nrm((DEPTH, NA_WIDTH, D), NA_WIDTH ** -0.5),
        'w_br_b': nrm((DEPTH, HY_WIDTH, D), HY_WIDTH ** -0.5),
        'w_out': nrm((DEPTH, D, D), D ** -0.5),
        'w_group': nrm((DEPTH, D, G), D ** -0.5),
        'b_group': nrm((DEPTH, G), 0.01),
        'w_router': nrm((DEPTH, D, G * E), D ** -0.5),
        'b_router': nrm((DEPTH, G * E), 0.01),
        'moe_w1': nrm((DEPTH, G, E, D, F), D ** -0.5),
        'moe_w3': nrm((DEPTH, G, E, D, F), D ** -0.5),
        'moe_w2': nrm((DEPTH, G, E, F, D), F ** -0.5),
    }


def reference(x, c, ctx, c_ctx, ada_w, ada_b, norm_mix, norm_ffn, w_in, q_norm, k_norm, rpb,
              short_w, short_b, flt_w1, flt_b1, flt_w2, flt_b2, flt_w3, hy_skip, w_br_a, w_br_b, w_out,
              w_group, b_group, w_router, b_router, moe_w1, moe_w3, moe_w2):
    xc = ctx
    for i in range(DEPTH):
        x, xc = _layer(x, xc, c, c_ctx, i == DEPTH - 1, ada_w[i], ada_b[i], norm_mix[i], norm_ffn[i],
                       w_in[i], q_norm[i], k_norm[i], rpb[i], short_w[i], short_b[i],
                       flt_w1[i], flt_b1[i], flt_w2[i], flt_b2[i], flt_w3[i], hy_skip[i],
                       w_br_a[i], w_br_b[i], w_out[i], w_group[i], b_group[i], w_router[i], b_router[i],
                       moe_w1[i], moe_w3[i], moe_w2[i])
    return x
```

```python
import os
import numpy as np
import ml_dtypes
import concourse.bass as bass
import concourse.mybir as mybir
from concourse.bass_utils import run_bass_kernel_spmd

F32 = mybir.dt.float32
BF16 = mybir.dt.bfloat16
I32 = mybir.dt.int32
AF = mybir.ActivationFunctionType
ALU = mybir.AluOpType
AX = mybir.AxisListType

SAME_ENGINE_SYNC = True
PE_T32 = os.environ.get('MK_PET32', '1') == '1'
NCORES = 8
D = 1024
L = 2048
LC = 256
NT = L + LC
DEPTH = 2
EPS = 1e-6
TB = [(0, 512, 0), (512, 512, 0), (1024, 512, 0), (1536, 512, 0), (2048, 256, 1)]
DBG_STAGE = os.environ.get("MK_STAGE", "")
DBG_COLS = 8192 if DBG_STAGE else 64


class Res:
    _n = 0

    def __init__(self, name=None):
        Res._n += 1
        self.name = (name or "r") + str(Res._n)
        self.writers = {}
        self.readers = {}
        self.slot = None


class Slot:
    def __init__(self, sem):
        self.sem = sem
        self.total = 0


class Op:
    __slots__ = ("fn", "deps", "signal", "dma_res", "dma_val")

    def __init__(self, fn, deps):
        self.fn = fn
        self.deps = deps
        self.signal = False
        self.dma_res = None
        self.dma_val = 0


class Prog:
    ENG = ("tensor", "vector", "scalar", "gpsimd", "sync")

    def __init__(self, nc):
        self.nc = nc
        self.ops = {e: [] for e in self.ENG}
        self.slots = []
        self.free_slots = []

    @staticmethod
    def _key(ref):
        return ref[1] if ref[0] == "e" else ("d", id(ref[1]))

    def _collect(self, eng, mykey, reads, writes, accs):
        deps = {}

        def add(ref, war=False):
            if ref[0] == "e" and ref[1] == eng and (eng == "tensor" or not SAME_ENGINE_SYNC or (war and eng == "vector")):
                return
            k = self._key(ref)
            if k not in deps or deps[k][2] < ref[2]:
                deps[k] = ref

        for r in reads:
            for ref in r.writers.values():
                add(ref)
        for w in writes:
            for ref in w.writers.values():
                add(ref)
            for ref in w.readers.values():
                add(ref, True)
        for w in accs:
            for k, ref in w.writers.items():
                if k != mykey:
                    add(ref)
            for ref in w.readers.values():
                add(ref, True)
        return list(deps.values())

    @staticmethod
    def _update(mykey, ref, reads, writes, accs):
        for w in writes:
            w.writers = {mykey: ref}
            w.readers = {}
        for w in accs:
            w.writers[mykey] = ref
        for r in reads:
            r.readers[mykey] = ref

    def op(self, eng, fn, reads=(), writes=(), accs=()):
        lst = self.ops[eng]
        deps = self._collect(eng, eng, reads, writes, accs)
        lst.append(Op(fn, deps))
        ref = ("e", eng, len(lst) - 1)
        self._update(eng, ref, reads, writes, accs)
        return ref

    def dma(self, out_ap, in_ap, owner, reads=(), writes=(), accs=(), eng="sync"):
        if owner.slot is None:
            if self.free_slots:
                owner.slot = self.free_slots.pop()
            else:
                owner.slot = Slot(self.nc.alloc_semaphore("dq%d" % len(self.slots)))
                self.slots.append(owner.slot)
        slot = owner.slot
        mykey = ("d", id(slot))
        deps = self._collect(eng, mykey, reads, writes, accs)
        slot.total += 16
        o = Op(lambda e: e.dma_start(out=out_ap, in_=in_ap), deps)
        o.dma_res = slot
        o.dma_val = slot.total
        self.ops[eng].append(o)
        ref = ("d", slot, slot.total)
        self._update(mykey, ref, reads, writes, accs)
        return ref

    def barrier(self):
        last = {}
        for e in self.ENG:
            if e != "sync" and self.ops[e]:
                last[e] = ("e", e, len(self.ops[e]) - 1)
        dmas = [("d", o, o.total) for o in self.slots if o.total > 0]
        for e in self.ENG:
            deps = [r for k, r in last.items() if k != e] + dmas
            self.ops[e].append(Op(None, deps))

    def build(self):
        nc = self.nc
        for e in self.ENG:
            for o in self.ops[e]:
                for d in o.deps:
                    if d[0] == "e":
                        self.ops[d[1]][d[2]].signal = True
        signum = {}
        for e in self.ENG:
            c = 0
            nums = []
            for o in self.ops[e]:
                if o.signal:
                    c += 1
                nums.append(c)
            signum[e] = nums
        esem = {e: nc.alloc_semaphore("e_" + e) for e in self.ENG if e != "sync"}

        def replay(ename):
            def run(engine):
                waited = {}
                for o in self.ops[ename]:
                    for d in o.deps:
                        if d[0] == "e":
                            sem = esem[d[1]]
                            val = signum[d[1]][d[2]]
                        else:
                            sem = d[1].sem
                            val = d[2]
                        sid = id(sem)
                        if waited.get(sid, 0) >= val:
                            continue
                        waited[sid] = val
                        engine.wait_ge(sem, val)
                    if o.fn is None:
                        if o.signal:
                            engine.nop().then_inc(esem[ename], 1)
                        continue
                    ins = o.fn(engine)
                    if o.dma_res is not None:
                        ins.then_inc(o.dma_res.sem, 16)
                    elif o.signal:
                        ins.then_inc(esem[ename], 1)
            return run

        with nc.Block() as block:
            block.tensor(replay("tensor"))
            block.vector(replay("vector"))
            block.scalar(replay("scalar"))
            block.gpsimd(replay("gpsimd"))
            block.sync(replay("sync"))


class T:
    def __init__(self, t, name):
        self.t = t
        self.r = Res(name)

    def __getitem__(self, k):
        return self.t[k]


def _dft_tables(Ls):
    N = 2 * Ls
    nt = Ls // 128
    s = np.arange(Ls, dtype=np.float64)
    w = 2.0 * np.pi * (np.arange(Ls, dtype=np.float64) + 0.5) / N
    ang = np.outer(s, w)
    out = {}
    for nm, M in (("C", np.cos(ang)), ("S", np.sin(ang))):
        F = M.reshape(nt, 128, nt, 128).transpose(2, 1, 0, 3)
        I = M.reshape(nt, 128, nt, 128).transpose(0, 3, 2, 1)
        out[nm + "F"] = np.ascontiguousarray(F).astype(ml_dtypes.bfloat16)
        out[nm + "I"] = np.ascontiguousarray(I).astype(ml_dtypes.bfloat16)
    return out


def _filter_consts(Ls):
    pos = np.arange(Ls, dtype=np.float32)
    t = pos / np.float32(max(Ls - 1, 1))
    w = (np.float32(2.0 * np.pi) * pos / np.float32(Ls)).astype(np.float32)
    f = np.linspace(1e-4, 15, 16, dtype=np.float32)
    arg = (f[None, :] * w[:, None]).astype(np.float32).astype(np.float64)
    z = np.concatenate([t[:, None].astype(np.float64), np.cos(arg), -np.sin(arg)], axis=-1)
    zT = np.ascontiguousarray(z.T).astype(np.float32)
    hmax = np.log(1e-2) / 0.3
    hmin = np.log(1e-2) / 1.5
    deltas = np.abs(np.linspace(hmin, hmax, 512, dtype=np.float32))
    dec = np.exp(-(t[:, None].astype(np.float32) * deltas[None, :]).astype(np.float32)).astype(np.float32)
    return zT, dec


def _rope_tables():
    pos = np.arange(L)
    rows, cols = pos // 64, pos % 64
    freqs = (100.0 ** (-np.arange(16, dtype=np.float32) / 16)).astype(np.float32)
    ar = (rows.astype(np.float32)[:, None] * freqs[None, :]).astype(np.float64)
    ac = (cols.astype(np.float32)[:, None] * freqs[None, :]).astype(np.float64)
    COS = np.concatenate([np.cos(ar), np.cos(ar), np.cos(ac), np.cos(ac)], axis=-1).astype(np.float32)
    SIN = np.concatenate([-np.sin(ar), np.sin(ar), -np.sin(ac), np.sin(ac)], axis=-1).astype(np.float32)
    return COS, SIN


ATT_CLS = []
for _i in range(16):
    if _i < 2:
        ATT_CLS.append((_i, [0, 1, 2, 3, 3]))
    elif _i < 14:
        ATT_CLS.append((2, [_i - 2, _i - 1, _i, _i + 1, _i + 2]))
    else:
        ATT_CLS.append((3 + (_i - 14), [12, 13, 14, 15, 15]))
_CLS_REP = {0: 0, 1: 1, 2: 2, 3: 14, 4: 15}
_CLS_VALID = {0: 4, 1: 4, 2: 5, 3: 4, 4: 4}


def _bias_tables(rpb):
    out = np.full((DEPTH, 5, 128, 8, 5, 128), -100.0, np.float32)
    qi = np.arange(128)
    ki = np.arange(128)
    for cls in range(5):
        i = _CLS_REP[cls]
        tiles = ATT_CLS[i][1]
        r = 2 * i + qi // 64
        col = qi % 64
        rs = np.clip(r - 4, 0, 24)
        ws = np.clip(col - 8, 0, 48)
        for c in range(_CLS_VALID[cls]):
            tok = tiles[c] * 128 + ki
            kr, kc = tok // 64, tok % 64
            inw = ((kr[:, None] >= rs[None, :]) & (kr[:, None] < rs[None, :] + 8) &
                   (kc[:, None] >= ws[None, :]) & (kc[:, None] < ws[None, :] + 16))
            dr = np.clip(kr[:, None] - r[None, :] + 7, 0, 14)
            dc = np.clip(kc[:, None] - col[None, :], -15, 15) + 15
            for l in range(DEPTH):
                g = rpb[l][:, dr, dc]
                g = np.where(inw[None], g, np.float32(-100.0))
                out[l, cls, :, :, c, :] = g.transpose(1, 0, 2)
    return out


class KB:
    def __init__(self):
        self.nc = bass.Bass("TRN2", target_bir_lowering=False)
        nc = self.nc
        self.P = Prog(nc)
        self.sb_lo = (nc.sbuf_base + 31) // 32 * 32
        self.sb_hi = nc.sbuf_top
        self.sp = self.sb_lo
        self.nalloc = 0
        self.live = []
        self.ps = [T(nc.alloc_psum_tensor(f"ps{i}", [128, 512], F32), f"ps{i}") for i in range(8)]
        self.ps_i = 0
        self.dram = {}

    def sb(self, name, shape, dt=F32):
        n = int(np.prod(shape[1:])) * (4 if dt in (F32, I32) else 2)
        n = (n + 31) // 32 * 32
        assert self.sp + n <= self.sb_hi, f"SBUF overflow allocating {name}: {self.sp + n - self.sb_lo}"
        self.nalloc += 1
        t = self.nc.alloc_sbuf_tensor_at(f"{name}_{self.nalloc}", list(shape), dt, offset=self.sp)
        tt_ = T(t, name)
        self.live.append((self.sp, tt_))
        self.sp += n
        return tt_

    def mark(self):
        return self.sp

    def release(self, m):
        self.P.barrier()
        keep = []
        for off, t in self.live:
            if off >= m:
                if t.r.slot is not None:
                    self.P.free_slots.append(t.r.slot)
                    t.r.slot = None
            else:
                keep.append((off, t))
        self.live = keep
        self.sp = m

    def psum(self):
        p = self.ps[self.ps_i % 6]
        self.ps_i += 1
        return p

    def din(self, name, shape, dt=F32):
        self.dram[name] = self.nc.dram_tensor(name, list(shape), dt, kind="ExternalInput").ap()
        return self.dram[name]

    def dscr(self, name, shape, dt=F32):
        return T(self.nc.dram_tensor(name, list(shape), dt).ap(), name)

    def mm(self, pt, pap, lhsT, rhs, start, stop, rd):
        self.P.op("tensor", lambda e: e.matmul(pap, lhsT=lhsT, rhs=rhs, start=start, stop=stop),
                  reads=rd, writes=[pt.r] if start else (), accs=() if start else [pt.r])

    def tr32(self, pt, pap, src, ident, rd, first):
        if PE_T32:
            self.P.op("tensor", lambda e: e.transpose(out=pap, in_=src, identity=ident),
                      reads=rd, writes=[pt.r] if first else (), accs=() if first else [pt.r])
        else:
            self.mm_new(pt, pap, src, ident, True, True, rd, first)

    def mm_new(self, pt, pap, lhsT, rhs, start, stop, rd, first):
        self.P.op("tensor", lambda e: e.matmul(pap, lhsT=lhsT, rhs=rhs, start=start, stop=stop),
                  reads=rd, writes=[pt.r] if first else (), accs=() if first else [pt.r])

    def act(self, out, in_, func, rd, wr=(), acc=(), **kw):
        self.P.op("scalar", lambda e: e.activation(out=out, in_=in_, func=func, **kw), reads=rd, writes=wr, accs=acc)

    def tt(self, eng, out, in0, in1, op, rd, wr=(), acc=()):
        self.P.op(eng, lambda e: e.tensor_tensor(out=out, in0=in0, in1=in1, op=op), reads=rd, writes=wr, accs=acc)

    def ts(self, eng, out, in0, s1, s2, op0, op1, rd, wr=(), acc=()):
        if s2 is None:
            self.P.op(eng, lambda e: e.tensor_scalar(out=out, in0=in0, scalar1=s1, scalar2=None, op0=op0), reads=rd, writes=wr, accs=acc)
        else:
            self.P.op(eng, lambda e: e.tensor_scalar(out=out, in0=in0, scalar1=s1, scalar2=s2, op0=op0, op1=op1), reads=rd, writes=wr, accs=acc)

    def stt(self, eng, out, in0, scalar, in1, op0, op1, rd, wr=(), acc=()):
        self.P.op(eng, lambda e: e.scalar_tensor_tensor(out=out, in0=in0, scalar=scalar, in1=in1, op0=op0, op1=op1),
                  reads=rd, writes=wr, accs=acc)

    def cp(self, eng, out, in_, rd, wr=(), acc=()):
        if eng == "scalar":
            self.P.op(eng, lambda e: e.copy(out=out, in_=in_), reads=rd, writes=wr, accs=acc)
        else:
            self.P.op(eng, lambda e: e.tensor_copy(out=out, in_=in_), reads=rd, writes=wr, accs=acc)

    def memset(self, eng, ap, val, wr=(), acc=()):
        self.P.op(eng, lambda e: e.memset(ap, val), writes=wr, accs=acc)

    def recip(self, out, in_, rd, wr=(), acc=()):
        self.P.op("vector", lambda e: e.reciprocal(out=out, in_=in_), reads=rd, writes=wr, accs=acc)

    def red(self, out, in_, op, rd, wr=(), acc=()):
        if op == "max":
            self.P.op("vector", lambda e: e.reduce_max(out=out, in_=in_, axis=AX.X), reads=rd, writes=wr, accs=acc)
        else:
            self.P.op("vector", lambda e: e.reduce_sum(out=out, in_=in_, axis=AX.X), reads=rd, writes=wr, accs=acc)

    def dma(self, out_ap, in_ap, owner, rd=(), wr=(), acc=()):
        self.P.dma(out_ap, in_ap, owner, reads=rd, writes=wr, accs=acc)

    def build(self):
        nc, P = self.nc, self.P
        din, sb = self.din, self.sb
        x_d = din("x", [2, L, D])
        ctx_d = din("ctx", [2, LC, D])
        cT_d = din("cT", [2, 128, 8, 2])
        ada_w = din("ada_w", [DEPTH, D, 6 * D])
        ada_bT = din("ada_bT", [DEPTH, 128, 48])
        nmT_d = din("norm_mixT", [DEPTH, 128, 8])
        nfT_d = din("norm_ffnT", [DEPTH, 128, 8])
        w_in = din("w_in", [DEPTH, D, 5120])
        qn_d = din("q_norm", [DEPTH, 64])
        kn_d = din("k_norm", [DEPTH, 64])
        biasT_d = din("biasT", [DEPTH, 5, 128, 8 * 5 * 128])
        swT_d = din("short_wT", [DEPTH, 128, 12, 3])
        sbT_d = din("short_bT", [DEPTH, 128, 12])
        fw1_d = din("flt_w1", [DEPTH, 33, 64])
        fb1_d = din("flt_b1", [DEPTH, 64, 1])
        fw2_d = din("flt_w2", [DEPTH, 64, 64])
        fb2_d = din("flt_b2", [DEPTH, 64, 1])
        fw3_d = din("flt_w3", [DEPTH, 64, 2048])
        skip_d = din("hy_skip", [DEPTH, 2, 512])
        wbra_d = din("w_br_a", [DEPTH, 512, D])
        wbrb_d = din("w_br_b", [DEPTH, 512, D])
        wout_d = din("w_out", [DEPTH, D, D])
        wgr_d = din("w_gr", [DEPTH, D, 36])
        bgr_d = din("b_gr", [DEPTH, 36])
        mw1_d = din("moe_w1", [DEPTH, 32, D, 256])
        mw3_d = din("moe_w3", [DEPTH, 32, D, 256])
        mw2_d = din("moe_w2", [DEPTH, 32, 256, D])
        ident_d = din("ident", [128, 128])
        cos_d = din("rope_cos", [L, 64])
        sin_d = din("rope_sin", [L, 64])
        sel_d = din("sel", [32, 32 * 128], BF16)
        tabs = {}
        for tag, Ls in (("m", L), ("c", LC)):
            nt = Ls // 128
            tabs[tag] = dict(
                L=Ls, nt=nt,
                zT=din(f"zT_{tag}", [33, Ls]), dec=din(f"dec_{tag}", [Ls, 512]),
                CF=din(f"CF_{tag}", [nt, 128, nt, 128], BF16), SF=din(f"SF_{tag}", [nt, 128, nt, 128], BF16),
                CI=din(f"CI_{tag}", [nt, 128, nt, 128], BF16), SI=din(f"SI_{tag}", [nt, 128, nt, 128], BF16),
                KF=[self.dscr(f"KF_{tag}{l}", [nt, 128, 2, 1024]) for l in range(DEPTH)],
            )
        out_d = self.nc.dram_tensor("out", [2, L, D], F32, kind="ExternalOutput").ap()
        out_r = Res("out")
        dbg_d = self.nc.dram_tensor("dbg", [128, DBG_COLS], F32, kind="ExternalOutput").ap()
        xscr = self.dscr("xscr", [128, 8, NT])
        escr = [[self.dscr(f"escr{l}_{c}", [128, 8 * 5 * 128], BF16) for c in range(5)] for l in range(DEPTH)]
        mixT = self.dscr("mixT", [128, 8, NT], BF16)
        gscr = self.dscr("gscr", [128, 16, NT], BF16)

        identf = sb("identf", [128, 128])
        identb = sb("identb", [128, 128], BF16)
        onesf = sb("onesf", [128, 128])
        onesb = sb("onesb", [128, 128], BF16)
        cst = sb("cst", [128, 4])
        nmT = sb("nmT", [128, DEPTH, 8])
        nfT = sb("nfT", [128, DEPTH, 8])
        scl = sb("scl", [128, 2, 6, 8])
        gainq = sb("gainq", [128, 8, 64])
        gaink = sb("gaink", [128, 8, 64])
        self.dma(identf[:], ident_d, identf.r, wr=[identf.r])
        self.cp("vector", identb[:], identf[:], [identf.r], wr=[identb.r])
        self.memset("vector", onesf[:], 1.0, wr=[onesf.r])
        self.memset("vector", onesb[:], 1.0, wr=[onesb.r])
        self.memset("vector", cst[:, 0:1], float(-np.pi), wr=[cst.r])
        self.memset("vector", cst[:, 1:2], EPS, acc=[cst.r])
        self.memset("vector", cst[:, 2:3], 0.0, acc=[cst.r])
        self.dma(nmT[:], nmT_d.rearrange("l p j -> p l j"), nmT.r, wr=[nmT.r])
        self.dma(nfT[:], nfT_d.rearrange("l p j -> p l j"), nfT.r, wr=[nfT.r])
        base_mark = self.mark()
        dbg_done = [False]

        def dump(src_ap, ncols, rd):
            st = sb("dbgst", [128, ncols])
            self.cp("vector", st[:], src_ap, rd, wr=[st.r])
            self.dma(dbg_d[:, 0:ncols], st[:], st.r, rd=[st.r], wr=[out_r])
            dbg_done[0] = True

        def phase_emask(l):
            m = self.mark()
            stg = [sb("est", [128, 5120]) for _ in range(2)]
            eb = [sb("eb", [128, 5120], BF16) for _ in range(2)]
            for c in range(5):
                s, b = stg[c % 2], eb[c % 2]
                self.dma(s[:], biasT_d[l, c], s.r, wr=[s.r])
                self.act(b[:], s[:], AF.Exp, [s.r], wr=[b.r])
                self.dma(escr[l][c][:], b[:], b.r, rd=[b.r], wr=[escr[l][c].r])
            self.release(m)

        def sin_rr(dst_ap, ps_t, ps_ap, bcol, n, tmps, wr, acc):
            u, ki, fr = tmps
            self.act(u[0:64, 0:n], ps_ap, AF.Identity, [ps_t.r, bcol.r], wr=[u.r], bias=bcol[0:64, 0:1], scale=float(1.0 / (2 * np.pi)))
            self.cp("vector", ki[0:64, 0:n], u[0:64, 0:n], [u.r], wr=[ki.r])
            self.tt("vector", fr[0:64, 0:n], u[0:64, 0:n], ki[0:64, 0:n], ALU.subtract, [u.r, ki.r], wr=[fr.r])
            self.stt("vector", u[0:64, 0:n], fr[0:64, 0:n], 0.0, fr[0:64, 0:n], ALU.is_lt, ALU.add, [fr.r], wr=[u.r])
            self.act(dst_ap, u[0:64, 0:n], AF.Sin, [u.r, cst.r], wr=wr, acc=acc, bias=cst[0:64, 0:1], scale=float(2 * np.pi))

        def phase_filter(l, tb):
            Ls, nt = tb["L"], tb["nt"]
            N = 2 * Ls
            m = self.mark()
            zT = sb("zT", [33, Ls]); w1 = sb("fw1", [33, 64]); w2 = sb("fw2", [64, 64]); w3 = sb("fw3", [64, 2048])
            b1 = sb("fb1", [64, 1]); b2 = sb("fb2", [64, 1])
            h1 = sb("h1", [64, Ls]); h2 = sb("h2", [64, Ls])
            tmps = (sb("u", [64, 512]), sb("ki", [64, 512], I32), sb("fr", [64, 512]))
            hd = sb("hd", [128, 4, 512]); ab = sb("ab", [128, 4, 512]); asum = sb("asum", [128, 2, 512])
            dec = [sb("dec", [128, 512]) for _ in range(2)]
            pm = sb("pm", [128, nt, 2, 1024], BF16)
            rn = sb("rn", [128, 2, 512])
            tc_ = [sb("tC", [128, nt, 128], BF16) for _ in range(2)]
            ts_ = [sb("tS", [128, nt, 128], BF16) for _ in range(2)]
            kst = [sb("kst", [128, 2, 1024]) for _ in range(2)]
            self.dma(zT[:], tb["zT"], zT.r, wr=[zT.r])
            self.dma(w1[:], fw1_d[l], w1.r, wr=[w1.r])
            self.dma(w2[:], fw2_d[l], w2.r, wr=[w2.r])
            self.dma(w3[:], fw3_d[l], w3.r, wr=[w3.r])
            self.dma(b1[:], fb1_d[l], b1.r, wr=[b1.r])
            self.dma(b2[:], fb2_d[l], b2.r, wr=[b2.r])
            for b in (b1, b2):
                self.ts("vector", b[:], b[:], float(1.0 / (2 * np.pi)), 8.5, ALU.mult, ALU.add, [b.r], wr=[b.r])
            nb = (Ls + 511) // 512
            for j in range(nb):
                n = min(512, Ls - j * 512)
                p = self.psum()
                self.mm(p, p[0:64, 0:n], w1[:], zT[:, j * 512:j * 512 + n], True, True, [w1.r, zT.r])
                sin_rr(h1[:, j * 512:j * 512 + n], p, p[0:64, 0:n], b1, n, tmps, (), [h1.r])
            for j in range(nb):
                n = min(512, Ls - j * 512)
                p = self.psum()
                self.mm(p, p[0:64, 0:n], w2[:], h1[:, j * 512:j * 512 + n], True, True, [w2.r, h1.r])
                sin_rr(h2[:, j * 512:j * 512 + n], p, p[0:64, 0:n], b2, n, tmps, (), [h2.r])
            pn = [self.ps[6], self.ps[7]]
            for lt in range(nt):
                dc_ = dec[lt % 2]
                self.dma(dc_[:], tb["dec"][lt * 128:(lt + 1) * 128, :], dc_.r, wr=[dc_.r])
                for cg in range(4):
                    p = self.psum()
                    self.mm(p, p[:, :], h2[:, lt * 128:(lt + 1) * 128], w3[:, cg * 512:(cg + 1) * 512], True, True, [h2.r, w3.r])
                    self.tt("vector", hd[:, cg, :], p[:, :], dc_[:], ALU.mult, [p.r, dc_.r], wr=[hd.r] if cg == 0 else (), acc=() if cg == 0 else [hd.r])
                if lt == 0:
                    self.memset("vector", hd[0:1, 2:4, :], 0.0, acc=[hd.r])
                self.act(ab[:], hd[:], AF.Abs, [hd.r], wr=[ab.r])
                self.tt("gpsimd", asum[:], ab[:, 0:2, :], ab[:, 2:4, :], ALU.add, [ab.r], wr=[asum.r])
                for o in range(2):
                    self.mm(pn[o], pn[o][:, :], onesf[:], asum[:, o, :], lt == 0, lt == nt - 1, [onesf.r, asum.r])
                self.tt("vector", pm[:, lt, 0, :].rearrange("p (o c) -> p o c", o=2), hd[:, 0:2, :], hd[:, 2:4, :], ALU.add, [hd.r], acc=[pm.r])
                self.tt("gpsimd", pm[:, lt, 1, :].rearrange("p (o c) -> p o c", o=2), hd[:, 2:4, :], hd[:, 0:2, :], ALU.subtract, [hd.r], acc=[pm.r])
            for o in range(2):
                self.ts("vector", rn[:, o, :], pn[o][:, :], EPS, float(N / 2.0), ALU.add, ALU.mult, [pn[o].r], wr=[rn.r] if o == 0 else (), acc=() if o == 0 else [rn.r])
            self.recip(rn[:], rn[:], [rn.r], wr=[rn.r])
            for fc in range(nt):
                tC, tS, ks = tc_[fc % 2], ts_[fc % 2], kst[fc % 2]
                self.dma(tC[:], tb["CF"][fc], tC.r, wr=[tC.r])
                self.dma(tS[:], tb["SF"][fc], tS.r, wr=[tS.r])
                for o in range(2):
                    pr, pi = self.psum(), self.psum()
                    for lt in range(nt):
                        self.mm(pr, pr[:, :], tC[:, lt, :], pm[:, lt, 0, o * 512:(o + 1) * 512], lt == 0, lt == nt - 1, [tC.r, pm.r])
                    for lt in range(nt):
                        self.mm(pi, pi[:, :], tS[:, lt, :], pm[:, lt, 1, o * 512:(o + 1) * 512], lt == 0, lt == nt - 1, [tS.r, pm.r])
                    first = (o == 0)
                    self.tt("vector", ks[:, 0, o * 512:(o + 1) * 512], pr[:, :], rn[:, o, :], ALU.mult, [pr.r, rn.r], wr=[ks.r] if first else (), acc=() if first else [ks.r])
                    self.tt("vector", ks[:, 1, o * 512:(o + 1) * 512], pi[:, :], rn[:, o, :], ALU.mult, [pi.r, rn.r], acc=[ks.r])
                self.dma(tb["KF"][l][fc], ks[:], ks.r, rd=[ks.r], acc=[tb["KF"][l].r])
            self.release(m)

        def phase_load(b):
            m = self.mark()
            xT = sb("xT", [128, 8, NT])
            stg = [sb("xst", [128, D]) for _ in range(2)]
            for t in range(18):
                s = stg[t % 2]
                src = x_d[b, t * 128:(t + 1) * 128, :] if t < 16 else ctx_d[b, (t - 16) * 128:(t - 15) * 128, :]
                self.dma(s[:], src, s.r, wr=[s.r])
                for half in range(2):
                    p = self.psum()
                    for j in range(4):
                        dc = half * 4 + j
                        self.tr32(p, p[:, j * 128:(j + 1) * 128], s[:, dc * 128:(dc + 1) * 128], identf[:], [s.r, identf.r], j == 0)
                    self.cp("scalar" if half else "vector", xT[:, half * 4:half * 4 + 4, t * 128:(t + 1) * 128],
                            p[:, :].rearrange("p (j c) -> p j c", j=4), [p.r], acc=[xT.r])
            self.dma(xscr[:], xT[:], xT.r, rd=[xT.r], wr=[xscr.r])
            self.release(m)

        def phase_ada(l, b):
            m = self.mark()
            craw = sb("craw", [128, 8, 2]); sT = sb("sT", [128, 8, 2])
            wst = [sb("adaw", [128, 8, 512]) for _ in range(2)]
            modT = sb("modT", [128, 48, 2]); adab = sb("adab", [128, 48]); tmp = sb("atmp", [128, 8])
            self.dma(craw[:], cT_d[b], craw.r, wr=[craw.r])
            self.dma(adab[:], ada_bT[l], adab.r, wr=[adab.r])
            self.act(sT[:], craw[:], AF.Silu, [craw.r], wr=[sT.r])
            pA = self.ps[6]
            for cg in range(12):
                w = wst[cg % 2]
                self.dma(w[:], ada_w[l][:, cg * 512:(cg + 1) * 512].rearrange("(dc p) c -> p dc c", p=128), w.r, wr=[w.r])
                for j4 in range(4):
                    j = cg * 4 + j4
                    for dc in range(8):
                        self.mm_new(pA, pA[:, j * 2:(j + 1) * 2], w[:, dc, j4 * 128:(j4 + 1) * 128], sT[:, dc, :], dc == 0, dc == 7, [w.r, sT.r], j == 0 and dc == 0)
            self.tt("vector", modT[:], pA[:, 0:96].rearrange("p (j s) -> p j s", s=2), adab[:].unsqueeze(2).to_broadcast([128, 48, 2]), ALU.add,
                    [pA.r, adab.r], wr=[modT.r])
            for s in range(2):
                first = (s == 0)
                for kind, (seg, nrm) in enumerate(((1, nmT), (0, None), (2, None), (4, nfT), (3, None), (5, None))):
                    src = modT[:, seg * 8:(seg + 1) * 8, s]
                    w_ = dict(wr=[scl.r]) if (first and kind == 0) else dict(acc=[scl.r])
                    if nrm is None:
                        self.cp("vector", scl[:, s, kind, :], src, [modT.r], **w_)
                    else:
                        self.ts("vector", tmp[:], src, 1.0, None, ALU.add, None, [modT.r], wr=[tmp.r])
                        self.tt("vector", scl[:, s, kind, :], tmp[:], nrm[:, l, :], ALU.mult, [tmp.r, nrm.r], **w_)
            self.dma(gainq[:], qn_d[l].partition_broadcast(128).unsqueeze(1).to_broadcast([128, 8, 64]), gainq.r, wr=[gainq.r])
            self.dma(gaink[:], kn_d[l].partition_broadcast(128).unsqueeze(1).to_broadcast([128, 8, 64]), gaink.r, wr=[gaink.r])
            self.ts("vector", gainq[:], gainq[:], 0.125, None, ALU.mult, None, [gainq.r], wr=[gainq.r])
            self.release(m)

        def norm_blocks(xT, outT, kindA, kindB, blocks, hook=None, f32buf=None):
            sq = [sb("sq", [128, 512], BF16) for _ in range(2)]
            rstd = sb("rstd", [128, 512])
            tmp = [sb("ntmp", [128, 512]) for _ in range(2)]
            pending = None
            for bi_, (t0, n, s) in enumerate(blocks):
                fb = f32buf[bi_ % 2] if f32buf is not None else None
                pS = self.psum()
                for dc in range(8):
                    q = sq[dc % 2]
                    self.act(q[:, 0:n], xT[:, dc, t0:t0 + n], AF.Square, [xT.r], wr=[q.r])
                    self.mm(pS, pS[:, 0:n], onesb[:], q[:, 0:n], dc == 0, dc == 7, [onesb.r, q.r])
                self.act(rstd[:, 0:n], pS[:, 0:n], AF.Sqrt, [pS.r, cst.r], wr=[rstd.r], scale=1.0 / D, bias=cst[:, 1:2])
                self.recip(rstd[:, 0:n], rstd[:, 0:n], [rstd.r], wr=[rstd.r])
                for dc in range(8):
                    tm = tmp[dc % 2]
                    self.stt("vector", tm[:, 0:n], xT[:, dc, t0:t0 + n], scl[:, s, kindA, dc:dc + 1], rstd[:, 0:n], ALU.mult, ALU.mult,
                             [xT.r, scl.r, rstd.r], wr=[tm.r])
                    if f32buf is None:
                        self.act(outT[:, dc, t0:t0 + n], tm[:, 0:n], AF.Identity, [tm.r, scl.r], acc=[outT.r], bias=scl[:, s, kindB, dc:dc + 1], scale=1.0)
                    else:
                        self.act(outT[:, dc, t0:t0 + n], tm[:, 0:n], AF.Identity, [tm.r, scl.r], acc=[outT.r], bias=scl[:, s, kindB, dc:dc + 1], scale=1.0)
                        self.ts("vector", fb[:, dc, 0:n], tm[:, 0:n], scl[:, s, kindB, dc:dc + 1], None, ALU.add, None, [tm.r, scl.r],
                                wr=[fb.r] if dc == 0 else (), acc=() if dc == 0 else [fb.r])
                if hook is not None:
                    if pending is not None:
                        hook(*pending)
                    pending = (t0, n, s, fb)
            if hook is not None and pending is not None:
                hook(*pending)

        def load_w_cols(dst, stg, src2d, c0, ncol):
            self.dma(stg[:, :, 0:ncol], src2d[:, c0:c0 + ncol].rearrange("(dc p) c -> p dc c", p=128), stg.r, wr=[stg.r])
            self.cp("gpsimd", dst[:, :, 0:ncol], stg[:, :, 0:ncol], [stg.r], wr=[dst.r])

        def transpose4(src_fn, nsrc, dst_ap, dst_t, rd, eng):
            p = self.psum()
            for j in range(nsrc):
                self.mm_new(p, p[:, j * 128:(j + 1) * 128], src_fn(j), identb[:], True, True, rd + [identb.r], j == 0)
            self.cp(eng, dst_ap, p[:, 0:nsrc * 128].rearrange("p (j c) -> p j c", j=nsrc), [p.r], acc=[dst_t.r])

        def layer_body(b, l, last):
            ntile = 16 if last else 18
            blocks_all = TB
            blocks_upd = TB[:4] if last else TB
            m0 = self.mark()
            phase_ada(l, b)
            hT = sb("hT", [128, 8, NT], BF16)
            mB = self.mark()
            xT = sb("xT", [128, 8, NT])
            self.dma(xT[:], xscr[:], xT.r, rd=[xscr.r], wr=[xT.r])
            norm_blocks(xT, hT, 0, 1, blocks_all)
            self.release(mB)
            if DBG_STAGE == "B":
                dump(hT[:, 0:2, 0:2304].rearrange("p a b -> p (a b)"), 4608, [hT.r]); return False
            qrT = sb("qrT", [128, 4, L], BF16); qpT = sb("qpT", [128, 4, NT], BF16); kT = sb("kT", [128, 4, NT], BF16)
            vaug = sb("vaug", [128, 18, 8, 65], BF16)
            mC = self.mark()
            wst = sb("wst", [128, 8, 512]); wbf = [sb("wbf", [128, 8, 512], BF16) for _ in range(2)]
            ropec = sb("ropec", [128, 16, 64]); ropes = sb("ropes", [128, 16, 64])
            self.dma(ropec[:], cos_d.rearrange("(t p) d -> p t d", p=128), ropec.r, wr=[ropec.r])
            self.dma(ropes[:], sin_d.rearrange("(t p) d -> p t d", p=128), ropes.r, wr=[ropes.r])
            qraw_ = [sb("qraw", [128, 512]) for _ in range(4)]
            sqt_ = [sb("sqt", [128, 512]) for _ in range(2)]; ssq_ = [sb("ssq", [128, 8]) for _ in range(3)]
            qn_ = [sb("qn", [128, 512]) for _ in range(2)]; qg_ = [sb("qg", [128, 512]) for _ in range(2)]
            t1_ = [sb("t1", [128, 512]) for _ in range(2)]; t2_ = [sb("t2", [128, 512]) for _ in range(2)]
            qpb_ = [sb("qpb", [128, 512], BF16) for _ in range(3)]; qrb_ = [sb("qrb", [128, 512], BF16) for _ in range(2)]
            self.memset("vector", vaug[:, :, :, 64:65], 1.0, wr=[vaug.r])
            v3 = lambda ap: ap.rearrange("p (h d) -> p h d", d=64)
            v5 = lambda ap: ap.rearrange("p (h r a d) -> p h r a d", h=8, r=2, a=2, d=16)
            items = [(0, t) for t in range(ntile)] + [(1, t) for t in range(18)]
            st_ = {}

            plain = lambda i: items[i][0] == 0 or items[i][1] >= 16
            rope = lambda i: items[i][1] < 16

            def s0(i):
                g, t = items[i]
                wb = wbf[g]
                if t == 0:
                    load_w_cols(wb, wst, w_in[l], g * 512, 512)
                p = self.psum()
                for dc in range(8):
                    self.mm(p, p[:, :], hT[:, dc, t * 128:(t + 1) * 128], wb[:, dc, :], dc == 0, dc == 7, [hT.r, wb.r])
                sqt, qraw = sqt_[i % 2], qraw_[i % 4]
                self.act(sqt[:], p[:, :], AF.Square, [p.r], wr=[sqt.r])
                self.cp("scalar", qraw[:], p[:, :], [p.r], wr=[qraw.r])

            def s1(i):
                sqt, ssq = sqt_[i % 2], ssq_[i % 3]
                self.red(ssq[:], v3(sqt[:]), "sum", [sqt.r], wr=[ssq.r])

            def s2(i):
                ssq = ssq_[i % 3]
                self.act(ssq[:], ssq[:], AF.Sqrt, [ssq.r, cst.r], wr=[ssq.r], scale=1.0 / 64, bias=cst[:, 1:2])

            def s3(i):
                ssq, qraw, qn = ssq_[i % 3], qraw_[i % 4], qn_[i % 2]
                self.recip(ssq[:], ssq[:], [ssq.r], wr=[ssq.r])
                self.tt("vector", v3(qn[:]), v3(qraw[:]), ssq[:].unsqueeze(2).to_broadcast([128, 8, 64]), ALU.mult, [qraw.r, ssq.r], wr=[qn.r])

            def s4(i):
                g, t = items[i]
                gain = gainq if g == 0 else gaink
                qn, qg = qn_[i % 2], qg_[i % 2]
                self.tt("gpsimd", v3(qg[:]), v3(qn[:]), gain[:], ALU.mult, [qn.r, gain.r], wr=[qg.r])

            def s5(i):
                g, t = items[i]
                qg, t1, t2, qpb = qg_[i % 2], t1_[i % 2], t2_[i % 2], qpb_[i % 3]
                if plain(i):
                    self.cp("scalar", qpb[:], qg[:], [qg.r], wr=[qpb.r])
                if rope(i):
                    self.tt("vector", v3(t1[:]), v3(qg[:]), ropec[:, t, :].unsqueeze(1).to_broadcast([128, 8, 64]), ALU.mult, [qg.r, ropec.r], wr=[t1.r])
                    sv = ropes[:, t, :].rearrange("p (r a d) -> p r a d", r=2, a=2, d=16)
                    self.tt("gpsimd", v5(t2[:])[:, :, :, 0, :], v5(qg[:])[:, :, :, 1, :], sv[:, :, 0, :].unsqueeze(1).to_broadcast([128, 8, 2, 16]), ALU.mult,
                            [qg.r, ropes.r], wr=[t2.r])
                    self.tt("gpsimd", v5(t2[:])[:, :, :, 1, :], v5(qg[:])[:, :, :, 0, :], sv[:, :, 1, :].unsqueeze(1).to_broadcast([128, 8, 2, 16]), ALU.mult,
                            [qg.r, ropes.r], acc=[t2.r])

            def s6(i):
                if rope(i):
                    t1, t2, qrb = t1_[i % 2], t2_[i % 2], qrb_[i % 2]
                    self.tt("vector", qrb[:], t1[:], t2[:], ALU.add, [t1.r, t2.r], wr=[qrb.r])

            def s7(i):
                g, t = items[i]
                outs = []
                if plain(i):
                    qpb = qpb_[i % 3]
                    p = self.psum()
                    for j in range(4):
                        self.mm_new(p, p[:, j * 128:(j + 1) * 128], qpb[:, j * 128:(j + 1) * 128], identb[:], True, True, [qpb.r, identb.r], j == 0)
                    outs.append((p, qpT if g == 0 else kT, "scalar"))
                if rope(i):
                    qrb = qrb_[i % 2]
                    p = self.psum()
                    for j in range(4):
                        self.mm_new(p, p[:, j * 128:(j + 1) * 128], qrb[:, j * 128:(j + 1) * 128], identb[:], True, True, [qrb.r, identb.r], j == 0)
                    outs.append((p, qrT if g == 0 else kT, "vector"))
                st_[i] = outs

            def s8(i):
                g, t = items[i]
                for (p, dstT, eng) in st_.pop(i):
                    self.cp(eng, dstT[:, :, t * 128:(t + 1) * 128], p[:, :].rearrange("p (j c) -> p j c", j=4), [p.r], acc=[dstT.r])

            stages = [s0, s1, s2, s3, s4, s5, s6, s7, s8]
            ni = len(items)
            for step in range(ni + len(stages) - 1):
                for k, fn in enumerate(stages):
                    i = step - k
                    if 0 <= i < ni:
                        fn(i)
            wb = wbf[0]
            load_w_cols(wb, wst, w_in[l], 2 * 512, 512)
            for t in range(18):
                p = self.psum()
                for dc in range(8):
                    self.mm(p, p[:, :], hT[:, dc, t * 128:(t + 1) * 128], wb[:, dc, :], dc == 0, dc == 7, [hT.r, wb.r])
                self.cp("scalar" if t % 2 else "vector", vaug[:, t, :, 0:64], v3(p[:, :]), [p.r], acc=[vaug.r])
            self.release(mC)
            if DBG_STAGE == "C1":
                dump(qrT[:, 0, 0:2048], 2048, [qrT.r]); return False
            emask = sb("emask", [128, 8, 5, 128], BF16)
            PT = [sb("PT", [128, 7, 128], BF16) for _ in range(4)]
            ao = sb("ao", [128, 8, 64], BF16); rec = sb("rec", [128, 8, 1])
            ast = [sb("ast", [128, 4, 128], BF16) for _ in range(2)]
            att = dict(cls=None, hc=0)
            pO = [self.ps[6], self.ps[7]]

            def att_S(i, h):
                main = i < 16
                if main:
                    cls, ktiles = ATT_CLS[i]
                    if h == 0 and cls != att["cls"]:
                        self.dma(emask[:].rearrange("p h c q -> p (h c q)"), escr[l][cls][:], emask.r, rd=[escr[l][cls].r], wr=[emask.r])
                        att["cls"] = cls
                pr_, hs = h // 2, (h % 2) * 64
                pt = PT[att["hc"] % 4]
                att["hc"] += 1
                qcols = slice(i * 128, (i + 1) * 128)
                pB = self.psum()
                if main:
                    pA = self.psum()
                    for c in range(4):
                        kt = ktiles[c]
                        self.mm_new(pA, pA[:, c * 128:(c + 1) * 128], kT[hs:hs + 64, pr_, kt * 128:(kt + 1) * 128], qrT[hs:hs + 64, pr_, qcols], True, True,
                                    [kT.r, qrT.r], c == 0)
                    kt = ktiles[4]
                    self.mm_new(pB, pB[:, 0:128], kT[hs:hs + 64, pr_, kt * 128:(kt + 1) * 128], qrT[hs:hs + 64, pr_, qcols], True, True, [kT.r, qrT.r], True)
                    for c in range(2):
                        self.mm_new(pB, pB[:, 128 + c * 128:256 + c * 128], kT[hs:hs + 64, pr_, L + c * 128:L + (c + 1) * 128], qpT[hs:hs + 64, pr_, qcols], True, True,
                                    [kT.r, qpT.r], False)
                    self.act(pt[:, 0:4, :], pA[:, :].rearrange("p (c q) -> p c q", c=4), AF.Exp, [pA.r], wr=[pt.r])
                    self.act(pt[:, 4:7, :], pB[:, 0:384].rearrange("p (c q) -> p c q", c=3), AF.Exp, [pB.r], acc=[pt.r])
                    self.tt("vector", pt[:, 0:5, :], pt[:, 0:5, :], emask[:, h, :, :], ALU.mult, [pt.r, emask.r], wr=[pt.r])
                    return pt, list(ktiles) + [16, 17], 7
                for c in range(2):
                    self.mm_new(pB, pB[:, c * 128:(c + 1) * 128], kT[hs:hs + 64, pr_, L + c * 128:L + (c + 1) * 128], qpT[hs:hs + 64, pr_, qcols], True, True,
                                [kT.r, qpT.r], c == 0)
                self.act(pt[:, 0:2, :], pB[:, 0:256].rearrange("p (c q) -> p c q", c=2), AF.Exp, [pB.r], wr=[pt.r])
                return pt, [16, 17], 2

            def att_PV(i, h, st):
                pt, vt, nch = st
                po = pO[h // 4]
                oc = (h % 4) * 65
                for c in range(nch):
                    self.mm_new(po, po[:, oc:oc + 65], pt[:, c, :], vaug[:, vt[c], h, :], c == 0, c == nch - 1, [pt.r, vaug.r], (h % 4 == 0) and c == 0)
                if h % 4 == 3:
                    pv = po[:, 0:260].rearrange("p (h e) -> p h e", e=65)
                    hh = (h // 4) * 4
                    self.recip(rec[:, hh:hh + 4, :], pv[:, :, 64:65], [po.r], acc=[rec.r])
                    self.tt("vector", ao[:, hh:hh + 4, :], pv[:, :, 0:64], rec[:, hh:hh + 4, :].to_broadcast([128, 4, 64]), ALU.mult, [po.r, rec.r], acc=[ao.r])
                if h == 7:
                    a_ = ast[i % 2]
                    aof = ao[:].rearrange("p h d -> p (h d)")
                    p = self.psum()
                    for j in range(4):
                        self.mm_new(p, p[:, j * 128:(j + 1) * 128], aof[:, j * 128:(j + 1) * 128], identb[:], True, True, [ao.r, identb.r], j == 0)
                    self.cp("scalar", a_[:], p[:, :].rearrange("p (j c) -> p j c", j=4), [p.r], wr=[a_.r])
                    self.dma(mixT[:, 0:4, i * 128:(i + 1) * 128], a_[:], a_.r, rd=[a_.r], acc=[mixT.r])

            pend = []
            for i in range(ntile):
                for h in range(8):
                    st = att_S(i, h)
                    pend.append((i, h, st))
                    if len(pend) > 2:
                        att_PV(*pend.pop(0))
            while pend:
                att_PV(*pend.pop(0))
            self.release(mB)
            if DBG_STAGE == "E":
                st = sb("dbgl", [128, 2048], BF16)
                self.dma(st[:], mixT[:, 0, 0:2048], st.r, rd=[mixT.r], wr=[st.r])
                dump(st[:], 2048, [st.r]); return False
            zv = sb("zv", [128, 18, 512], BF16); zx1 = sb("zx1", [128, 18, 512], BF16); zx2 = sb("zx2", [128, 18, 512], BF16)
            zs = [zv, zx1, zx2]
            mC2 = self.mark()
            wst = sb("wst", [128, 8, 512]); wbf = [sb("wbf", [128, 8, 512], BF16) for _ in range(2)]
            U_ = [sb("U", [128, NT + 4]) for _ in range(3)]; cv_ = [sb("cv", [128, NT]) for _ in range(2)]; cvb_ = [sb("cvb", [128, NT], BF16) for _ in range(2)]
            swT = sb("swT", [128, 12, 3]); sbT = sb("sbT", [128, 12])
            gst = [sb("gst", [128, 4, 512], BF16) for _ in range(2)]
            self.dma(swT[:], swT_d[l], swT.r, wr=[swT.r])
            self.dma(sbT[:], sbT_d[l], sbT.r, wr=[sbT.r])
            for U in U_:
                self.memset("vector", U[:], 0.0, wr=[U.r])
            segs = [(0, L, 1)] + ([] if last else [(L, LC, 3)])
            blocks_h = TB[:4] if last else TB
            def c2_T0(k):
                g, cc = divmod(k, 4)
                wb = wbf[g % 2]
                if cc == 0:
                    load_w_cols(wb, wst, w_in[l], 1536 + g * 512, 512)
                U = U_[k % 3]
                for bi, (t0, n, s) in enumerate(blocks_h):
                    p = self.psum()
                    for dc in range(8):
                        self.mm(p, p[:, 0:n], wb[:, dc, cc * 128:(cc + 1) * 128], hT[:, dc, t0:t0 + n], dc == 0, dc == 7, [wb.r, hT.r])
                    uo = t0 + (1 if s == 0 else 3)
                    self.cp("scalar" if bi % 2 else "vector", U[:, uo:uo + n], p[:, 0:n], [p.r], acc=[U.r])

            def c2_T1(k):
                U, cv = U_[k % 3], cv_[k % 2]
                for (s0, n, sh) in segs:
                    u0 = s0 + sh
                    self.act(cv[:, s0:s0 + n], U[:, u0:u0 + n], AF.Identity, [U.r, swT.r, sbT.r], acc=[cv.r], scale=swT[:, k, 1:2], bias=sbT[:, k:k + 1])

            def c2_T2(k):
                U, cv, cvb = U_[k % 3], cv_[k % 2], cvb_[k % 2]
                for (s0, n, sh) in segs:
                    u0 = s0 + sh
                    self.stt("vector", cv[:, s0:s0 + n], U[:, u0 - 1:u0 - 1 + n], swT[:, k, 0:1], cv[:, s0:s0 + n], ALU.mult, ALU.add, [U.r, swT.r, cv.r], acc=[cv.r])
                    self.stt("vector", cvb[:, s0:s0 + n], U[:, u0 + 1:u0 + 1 + n], swT[:, k, 2:3], cv[:, s0:s0 + n], ALU.mult, ALU.add, [U.r, swT.r, cv.r], acc=[cvb.r])

            def c2_T3(k):
                g, cc = divmod(k, 4)
                cvb = cvb_[k % 2]
                for t4 in range(0, ntile, 4):
                    nn = min(4, ntile - t4)
                    transpose4(lambda j: cvb[:, (t4 + j) * 128:(t4 + j + 1) * 128], nn, zs[g][:, t4:t4 + nn, cc * 128:(cc + 1) * 128], zs[g], [cvb.r],
                               "scalar" if (t4 // 4) % 2 else "vector")

            c2st = [c2_T0, c2_T1, c2_T2, c2_T3]
            for step in range(12 + 3):
                for kk, fn in enumerate(c2st):
                    i = step - kk
                    if 0 <= i < 12:
                        fn(i)
            for g in range(4):
                wb = wbf[g % 2]
                load_w_cols(wb, wst, w_in[l], 3072 + g * 512, 512)
                for bi, (t0, n, s) in enumerate(blocks_h):
                    gs = gst[bi % 2]
                    for cc in range(4):
                        p = self.psum()
                        for dc in range(8):
                            self.mm(p, p[:, 0:n], wb[:, dc, cc * 128:(cc + 1) * 128], hT[:, dc, t0:t0 + n], dc == 0, dc == 7, [wb.r, hT.r])
                        self.act(gs[:, cc, 0:n], p[:, 0:n], AF.Sigmoid, [p.r], wr=[gs.r] if cc == 0 else (), acc=() if cc == 0 else [gs.r])
                    self.dma(gscr[:, g * 4:(g + 1) * 4, t0:t0 + n], gs[:, :, 0:n], gs.r, rd=[gs.r], acc=[gscr.r])
            self.release(mC2)
            if DBG_STAGE == "C2":
                dump(zv[:, 0:4, :].rearrange("p a b -> p (a b)"), 2048, [zv.r]); return False
            skipB = sb("skipB", [128, 2, 512])
            self.dma(skipB[:], skip_d[l].partition_broadcast(128), skipB.r, wr=[skipB.r])
            Y = sb("Y", [128, 16, 2, 512], BF16)
            tc_ = [sb("tC", [128, 16, 128], BF16) for _ in range(2)]
            ts_ = [sb("tS", [128, 16, 128], BF16) for _ in range(2)]
            kf = [sb("kf", [128, 2, 512]) for _ in range(2)]
            ht = [sb("ht", [128, 512]) for _ in range(4)]
            hst = [sb("hst", [128, 4, 128], BF16) for _ in range(2)]
            streams = [(tabs["m"], 0)] + ([] if last else [(tabs["c"], 16)])
            for tb, tile0 in streams:
                nt = tb["nt"]
                for o in range(2):
                    zin = zs[o]
                    gate = zs[o + 1]
                    for fc in range(nt):
                        tC, tS, kk = tc_[fc % 2], ts_[fc % 2], kf[fc % 2]
                        self.dma(tC[:, 0:nt, :], tb["CF"][fc], tC.r, wr=[tC.r])
                        self.dma(tS[:, 0:nt, :], tb["SF"][fc], tS.r, wr=[tS.r])
                        self.dma(kk[:], tb["KF"][l][fc, :, :, o * 512:(o + 1) * 512], kk.r, rd=[tb["KF"][l].r], wr=[kk.r])
                        pr, pi = self.psum(), self.psum()
                        for st in range(nt):
                            self.mm(pr, pr[:, :], tC[:, st, :], zin[:, tile0 + st, :], st == 0, st == nt - 1, [tC.r, zin.r])
                        for st in range(nt):
                            self.mm(pi, pi[:, :], tS[:, st, :], zin[:, tile0 + st, :], st == 0, st == nt - 1, [tS.r, zin.r])
                        self.tt("vector", ht[0][:], pr[:, :], kk[:, 0, :], ALU.mult, [pr.r, kk.r], wr=[ht[0].r])
                        self.tt("vector", ht[1][:], pi[:, :], kk[:, 1, :], ALU.mult, [pi.r, kk.r], wr=[ht[1].r])
                        self.tt("gpsimd", Y[:, fc, 0, :], ht[0][:], ht[1][:], ALU.add, [ht[0].r, ht[1].r], acc=[Y.r])
                        self.tt("vector", ht[2][:], pi[:, :], kk[:, 0, :], ALU.mult, [pi.r, kk.r], wr=[ht[2].r])
                        self.tt("vector", ht[3][:], pr[:, :], kk[:, 1, :], ALU.mult, [pr.r, kk.r], wr=[ht[3].r])
                        self.tt("gpsimd", Y[:, fc, 1, :], ht[2][:], ht[3][:], ALU.subtract, [ht[2].r, ht[3].r], acc=[Y.r])
                    for tt_ in range(nt):
                        tC, tS = tc_[tt_ % 2], ts_[tt_ % 2]
                        self.dma(tC[:, 0:nt, :], tb["CI"][tt_], tC.r, wr=[tC.r])
                        self.dma(tS[:, 0:nt, :], tb["SI"][tt_], tS.r, wr=[tS.r])
                        p = self.psum()
                        for fc in range(nt):
                            self.mm(p, p[:, :], tC[:, fc, :], Y[:, fc, 0, :], fc == 0, False, [tC.r, Y.r])
                            self.mm(p, p[:, :], tS[:, fc, :], Y[:, fc, 1, :], False, fc == nt - 1, [tS.r, Y.r])
                        tl = tile0 + tt_
                        a0, a1 = ht[tt_ % 2], ht[2 + tt_ % 2]
                        self.tt("gpsimd", a0[:], zin[:, tl, :], skipB[:, o, :], ALU.mult, [zin.r, skipB.r], wr=[a0.r])
                        self.tt("vector", a1[:], p[:, :], a0[:], ALU.add, [p.r, a0.r], wr=[a1.r])
                        self.tt("vector", gate[:, tl, :], a1[:], gate[:, tl, :], ALU.mult, [a1.r, gate.r], acc=[gate.r])
                for t in range(tile0, tile0 + nt):
                    h_ = hst[t % 2]
                    p = self.psum()
                    for j in range(4):
                        self.mm_new(p, p[:, j * 128:(j + 1) * 128], zx2[:, t, j * 128:(j + 1) * 128], identb[:], True, True, [zx2.r, identb.r], j == 0)
                    self.cp("scalar", h_[:], p[:, :].rearrange("p (j c) -> p j c", j=4), [p.r], wr=[h_.r])
                    self.dma(mixT[:, 4:8, t * 128:(t + 1) * 128], h_[:], h_.r, rd=[h_.r], acc=[mixT.r])
            self.release(m0)
            if DBG_STAGE == "H":
                st = sb("dbgl", [128, 2048], BF16)
                self.dma(st[:], mixT[:, 4, 0:2048], st.r, rd=[mixT.r], wr=[st.r])
                dump(st[:], 2048, [st.r]); return False
            xT = sb("xT", [128, 8, NT])
            mM = self.mark()
            wa = sb("wa", [128, 4, D], BF16); wbb = sb("wbb", [128, 4, D], BF16); wo = sb("wo", [128, 8, D], BF16)
            wst4_ = [sb("wst4", [128, 4, D]) for _ in range(2)]
            mci = 0
            def m_load(dst, src, r0, mci):
                wst4 = wst4_[mci % 2]
                self.dma(wst4[:], src[r0:r0 + 512, :].rearrange("(a p) c -> p a c", p=128), wst4.r, wr=[wst4.r])
                ceng = ("gpsimd", "vector", "scalar", "gpsimd")[mci % 4]
                if r0 == 0:
                    self.cp(ceng, dst[:, 0:4, :], wst4[:], [wst4.r], wr=[dst.r])
                else:
                    self.cp(ceng, dst[:, 4:8, :], wst4[:], [wst4.r], acc=[dst.r])

            m_load(wa, wbra_d[l], 0, 0)
            m_load(wbb, wbrb_d[l], 0, 1)
            mixb = [sb("mixb", [128, 8, 512], BF16) for _ in range(2)]
            gb = [sb("gb", [128, 2, 512], BF16) for _ in range(4)]
            yT = sb("yT", [128, 8, 512], BF16)
            ta = [sb("ta", [128, 512]) for _ in range(2)]; tbm = [sb("tbm", [128, 512]) for _ in range(2)]
            for bi, (t0, n, s) in enumerate(blocks_upd):
                mb = mixb[bi % 2]
                self.dma(mb[:, :, 0:n], mixT[:, :, t0:t0 + n], mb.r, rd=[mixT.r], wr=[mb.r])
                for yc in range(8):
                    g_ = gb[yc % 4]
                    self.dma(g_[:, 0, 0:n], gscr[:, yc, t0:t0 + n], g_.r, rd=[gscr.r], wr=[g_.r])
                    self.dma(g_[:, 1, 0:n], gscr[:, 8 + yc, t0:t0 + n], g_.r, rd=[gscr.r], acc=[g_.r])
                    if bi == 0 and yc == 1:
                        m_load(wo, wout_d[l], 0, 2)
                    if bi == 0 and yc == 2:
                        m_load(wo, wout_d[l], 512, 3)
                    if bi == 0 and yc == 3:
                        self.dma(xT[:], xscr[:], xT.r, rd=[xscr.r], wr=[xT.r])
                    pa, pb = self.psum(), self.psum()
                    for ac in range(4):
                        self.mm(pa, pa[:, 0:n], wa[:, ac, yc * 128:(yc + 1) * 128], mb[:, ac, 0:n], ac == 0, ac == 3, [wa.r, mb.r])
                    for ac in range(4):
                        self.mm(pb, pb[:, 0:n], wbb[:, ac, yc * 128:(yc + 1) * 128], mb[:, 4 + ac, 0:n], ac == 0, ac == 3, [wbb.r, mb.r])
                    a_, b_ = ta[yc % 2], tbm[yc % 2]
                    self.tt("vector", a_[:, 0:n], pa[:, 0:n], g_[:, 0, 0:n], ALU.mult, [pa.r, g_.r], wr=[a_.r])
                    self.tt("vector", b_[:, 0:n], pb[:, 0:n], g_[:, 1, 0:n], ALU.mult, [pb.r, g_.r], wr=[b_.r])
                    self.tt("gpsimd", yT[:, yc, 0:n], a_[:, 0:n], b_[:, 0:n], ALU.add, [a_.r, b_.r], wr=[yT.r] if yc == 0 else (), acc=() if yc == 0 else [yT.r])
                for dc in range(8):
                    p = self.psum()
                    for yc in range(8):
                        self.mm(p, p[:, 0:n], wo[:, yc, dc * 128:(dc + 1) * 128], yT[:, yc, 0:n], yc == 0, yc == 7, [wo.r, yT.r])
                    self.stt("vector", xT[:, dc, t0:t0 + n], p[:, 0:n], scl[:, s, 2, dc:dc + 1], xT[:, dc, t0:t0 + n], ALU.mult, ALU.add, [p.r, scl.r, xT.r], acc=[xT.r])
            self.release(mM)
            if DBG_STAGE == "M":
                dump(xT[:, 0, 0:2304], 2304, [xT.r]); return False
            h2T = sb("h2T", [128, 8, NT], BF16)
            cwT = sb("cwT", [32, NT], BF16)
            mN = self.mark()
            h2f_ = [sb("h2f", [128, 8, 512]) for _ in range(2)]
            wr_ = sb("wr", [128, 8, 36]); brB = sb("brB", [128, 36])
            lg = sb("lg", [128, 18, 36]); sm = sb("sm", [128, 12, 18]); ohg = sb("ohg", [128, 18, 4]); eg = sb("eg", [128, 18, 4])
            em = sb("em", [128, 18, 32]); em2 = sb("em2", [128, 18, 32]); oh1 = sb("oh1", [128, 18, 32]); oh2 = sb("oh2", [128, 18, 32])
            cwb = sb("cwb", [128, 18, 32], BF16)
            self.dma(wr_[:], wgr_d[l].rearrange("(dc p) c -> p dc c", p=128), wr_.r, wr=[wr_.r])
            self.dma(brB[:], bgr_d[l].partition_broadcast(128), brB.r, wr=[brB.r])

            NTL = 16 if last else 18
            pR = [self.ps[6], self.ps[7]]

            def route(t0, n, s, h2f):
                for tt_ in range(n // 128):
                    tl = t0 // 128 + tt_
                    pr = pR[tl // 9]
                    co = (tl % 9) * 36
                    c0 = tt_ * 128
                    for dc in range(8):
                        self.mm_new(pr, pr[:, co:co + 36], h2f[:, dc, c0:c0 + 128], wr_[:, dc, :], dc == 0, dc == 7, [h2f.r, wr_.r], (tl % 9 == 0) and dc == 0)

            def route_all():
                nt_ = NTL
                B3 = lambda ap, k: ap.unsqueeze(2).to_broadcast([128, nt_, k])
                for hb in range(2):
                    na = min(9, nt_ - hb * 9)
                    self.tt("vector", lg[:, hb * 9:hb * 9 + na, :], pR[hb][:, 0:na * 36].rearrange("p (t c) -> p t c", c=36),
                            brB[:].unsqueeze(1).to_broadcast([128, na, 36]), ALU.add, [pR[hb].r, brB.r], wr=[lg.r] if hb == 0 else (), acc=() if hb == 0 else [lg.r])
                LG, LE = lg[:, 0:nt_, 0:4], lg[:, 0:nt_, 4:36]
                S = lambda k: sm[:, k, 0:nt_]
                self.red(S(0), LG, "max", [lg.r], wr=[sm.r])
                self.tt("vector", ohg[:, 0:nt_, :], LG, B3(S(0), 4), ALU.is_equal, [lg.r, sm.r], wr=[ohg.r])
                self.tt("vector", eg[:, 0:nt_, :], LG, B3(S(0), 4), ALU.subtract, [lg.r, sm.r], wr=[eg.r])
                self.act(eg[:, 0:nt_, :], eg[:, 0:nt_, :], AF.Exp, [eg.r], wr=[eg.r])
                self.red(S(2), eg[:, 0:nt_, :], "sum", [eg.r], acc=[sm.r])
                self.recip(S(3), S(2), [sm.r], acc=[sm.r])
                self.ts("vector", ohg[:, 0:nt_, :], ohg[:, 0:nt_, :], 1.0, 1e9, ALU.subtract, ALU.mult, [ohg.r], wr=[ohg.r])
                self.tt("vector", em[:, 0:nt_, :].rearrange("p t (g e) -> p t g e", g=4), LE.rearrange("p t (g e) -> p t g e", g=4),
                        ohg[:, 0:nt_, :].unsqueeze(3).to_broadcast([128, nt_, 4, 8]), ALU.add, [lg.r, ohg.r], wr=[em.r])
                self.red(S(4), em[:, 0:nt_, :], "max", [em.r], acc=[sm.r])
                self.tt("vector", oh1[:, 0:nt_, :], em[:, 0:nt_, :], B3(S(4), 32), ALU.is_equal, [em.r, sm.r], wr=[oh1.r])
                self.stt("vector", em2[:, 0:nt_, :], oh1[:, 0:nt_, :], -1e9, em[:, 0:nt_, :], ALU.mult, ALU.add, [oh1.r, em.r], wr=[em2.r])
                self.red(S(5), em2[:, 0:nt_, :], "max", [em2.r], acc=[sm.r])
                self.tt("vector", oh2[:, 0:nt_, :], em2[:, 0:nt_, :], B3(S(5), 32), ALU.is_equal, [em2.r, sm.r], wr=[oh2.r])
                self.tt("vector", S(6), S(5), S(4), ALU.subtract, [sm.r], acc=[sm.r])
                self.act(S(7), S(6), AF.Exp, [sm.r], acc=[sm.r])
                self.ts("vector", S(8), S(7), 1.0, None, ALU.add, None, [sm.r], acc=[sm.r])
                self.recip(S(9), S(8), [sm.r], acc=[sm.r])
                self.tt("vector", S(10), S(7), S(9), ALU.mult, [sm.r], acc=[sm.r])
                self.tt("vector", S(9), S(9), S(3), ALU.mult, [sm.r], acc=[sm.r])
                self.tt("vector", S(10), S(10), S(3), ALU.mult, [sm.r], acc=[sm.r])
                self.tt("vector", oh1[:, 0:nt_, :], oh1[:, 0:nt_, :], B3(S(9), 32), ALU.mult, [oh1.r, sm.r], wr=[oh1.r])
                self.tt("vector", oh2[:, 0:nt_, :], oh2[:, 0:nt_, :], B3(S(10), 32), ALU.mult, [oh2.r, sm.r], wr=[oh2.r])
                self.tt("vector", cwb[:, 0:nt_, :], oh1[:, 0:nt_, :], oh2[:, 0:nt_, :], ALU.add, [oh1.r, oh2.r], wr=[cwb.r])
                for t4 in range(0, nt_, 4):
                    nn = min(4, nt_ - t4)
                    p2 = self.psum()
                    for j in range(nn):
                        self.mm_new(p2, p2[0:32, j * 128:(j + 1) * 128], cwb[:, t4 + j, :], identb[:], True, True, [cwb.r, identb.r], j == 0)
                    self.cp("scalar" if (t4 // 4) % 2 else "vector", cwT[:, t4 * 128:(t4 + nn) * 128], p2[0:32, 0:nn * 128], [p2.r], acc=[cwT.r])

            norm_blocks(xT, h2T, 3, 4, blocks_upd, hook=route, f32buf=h2f_)
            route_all()
            self.release(mN)
            if DBG_STAGE == "N":
                dump(cwT[:, 0:2304], 2304, [cwT.r]); return False
            sel = sb("sel", [32, 32 * 128], BF16)
            self.dma(sel[:], sel_d, sel.r, wr=[sel.r])
            stg = [sb("wstg", [128, 1024]) for _ in range(2)]
            wb1 = [[sb("wb1", [128, 8, 256], BF16) for _ in range(2)] for _ in range(2)]
            wb3 = [[sb("wb3", [128, 8, 256], BF16) for _ in range(2)] for _ in range(2)]
            wb2 = [[sb("wb2", [128, 2, D], BF16) for _ in range(2)] for _ in range(2)]
            sa = [sb("sa", [128, 512]) for _ in range(4)]
            cws = [[sb("cws", [128, 512], BF16) for _ in range(2)] for _ in range(2)]
            hid = [sb("hid", [128, 4, 512], BF16) for _ in range(2)]
            cnt = 0
            ycnt = 0
            kst_ = 0
            for ep in range(16):
                pbuf = ep % 2
                for j in range(2):
                    e = 2 * ep + j
                    b1_, b3_, b2_ = wb1[pbuf][j], wb3[pbuf][j], wb2[pbuf][j]
                    for (dst, src) in ((b1_, mw1_d[l, e]), (b3_, mw3_d[l, e])):
                        v = src.rearrange("(dc p) f -> p dc f", p=128)
                        for hh in range(2):
                            sg = stg[kst_ % 2]
                            kst_ += 1
                            self.dma(sg[:].rearrange("p (a f) -> p a f", a=4), v[:, hh * 4:(hh + 1) * 4, :], sg.r, wr=[sg.r])
                            self.cp("gpsimd", dst[:, hh * 4:(hh + 1) * 4, :], sg[:].rearrange("p (a f) -> p a f", a=4), [sg.r],
                                    wr=[dst.r] if hh == 0 else (), acc=() if hh == 0 else [dst.r])
                    v = mw2_d[l, e].rearrange("(fc p) d -> p fc d", p=128)
                    for hh in range(2):
                        sg = stg[kst_ % 2]
                        kst_ += 1
                        self.dma(sg[:], v[:, hh, :], sg.r, wr=[sg.r])
                        self.cp("gpsimd", b2_[:, hh, :], sg[:], [sg.r], wr=[b2_.r] if hh == 0 else (), acc=() if hh == 0 else [b2_.r])
                for (t0, n, s) in blocks_upd:
                    hd_ = hid[cnt % 2]
                    cw_ = cws[cnt % 2]
                    cnt += 1
                    for j in range(2):
                        e = 2 * ep + j
                        pc = self.ps[ycnt % 8]
                        ycnt += 1
                        self.mm(pc, pc[:, 0:n], sel[:, e * 128:(e + 1) * 128], cwT[:, t0:t0 + n], True, True, [sel.r, cwT.r])
                        self.cp("scalar", cw_[j][:, 0:n], pc[:, 0:n], [pc.r], wr=[cw_[j].r])
                    for j in range(2):
                        b1_, b3_ = wb1[pbuf][j], wb3[pbuf][j]
                        for fc in range(2):
                            k = j * 2 + fc
                            pa, pb = self.ps[ycnt % 8], self.ps[(ycnt + 1) % 8]
                            ycnt += 2
                            for dc in range(8):
                                self.mm(pa, pa[:, 0:n], b1_[:, dc, fc * 128:(fc + 1) * 128], h2T[:, dc, t0:t0 + n], dc == 0, dc == 7, [b1_.r, h2T.r])
                            for dc in range(8):
                                self.mm(pb, pb[:, 0:n], b3_[:, dc, fc * 128:(fc + 1) * 128], h2T[:, dc, t0:t0 + n], dc == 0, dc == 7, [b3_.r, h2T.r])
                            s_ = sa[k]
                            self.act(s_[:, 0:n], pa[:, 0:n], AF.Silu, [pa.r], wr=[s_.r])
                            self.tt("vector", s_[:, 0:n], pb[:, 0:n], s_[:, 0:n], ALU.mult, [pb.r, s_.r], wr=[s_.r])
                            self.tt("vector", hd_[:, k, 0:n], cw_[j][:, 0:n], s_[:, 0:n], ALU.mult, [cw_[j].r, s_.r], wr=[hd_.r] if k == 0 else (), acc=() if k == 0 else [hd_.r])
                    for dc in range(8):
                        p = self.ps[ycnt % 8]
                        ycnt += 1
                        for k in range(4):
                            j, fc = divmod(k, 2)
                            self.mm(p, p[:, 0:n], wb2[pbuf][j][:, fc, dc * 128:(dc + 1) * 128], hd_[:, k, 0:n], k == 0, k == 3, [wb2[pbuf][j].r, hd_.r])
                        self.stt("vector", xT[:, dc, t0:t0 + n], p[:, 0:n], scl[:, s, 5, dc:dc + 1], xT[:, dc, t0:t0 + n], ALU.mult, ALU.add, [p.r, scl.r, xT.r], acc=[xT.r])
            if DBG_STAGE == "X":
                dump(xT[:, 0, 0:2304], 2304, [xT.r]); return False
            if not last:
                self.dma(xscr[:], xT[:], xT.r, rd=[xT.r], wr=[xscr.r])
            else:
                self.release(mN)
                ost = [sb("ost", [128, D]) for _ in range(2)]
                for t in range(16):
                    o_ = ost[t % 2]
                    for half in range(2):
                        p = self.psum()
                        for j in range(4):
                            dc = half * 4 + j
                            self.tr32(p, p[:, j * 128:(j + 1) * 128], xT[:, dc, t * 128:(t + 1) * 128], identf[:], [xT.r, identf.r], j == 0)
                        self.cp("scalar" if half else "vector", o_[:, half * 512:(half + 1) * 512], p[:, :], [p.r], wr=[o_.r] if half == 0 else (), acc=() if half == 0 else [o_.r])
                    self.dma(out_d[b, t * 128:(t + 1) * 128, :], o_[:], o_.r, rd=[o_.r], acc=[out_r])
            self.release(m0)
            return True

        nb = int(os.environ.get("MK_NB", "2"))
        nl = int(os.environ.get("MK_NL", str(DEPTH)))
        ok = True
        for b in range(nb):
            phase_load(b)
            for l in range(nl):
                if b == 0:
                    phase_emask(l)
                    phase_filter(l, tabs["m"])
                    if l != DEPTH - 1:
                        phase_filter(l, tabs["c"])
                ok = layer_body(b, l, l == DEPTH - 1)
                if not ok:
                    break
            if not ok:
                break
        self.P.barrier()
        if not dbg_done[0]:
            st = sb("dbgz", [128, 64])
            self.memset("vector", st[:], 0.0, wr=[st.r])
            self.dma(dbg_d[:, 0:64], st[:], st.r, rd=[st.r], wr=[Res("dbgo")])
            self.P.barrier()
        self.P.build()
        return self.nc


_CACHE = {}


def _host_consts():
    if "c" in _CACHE:
        return _CACHE["c"]
    c = {}
    c["ident"] = np.eye(128, dtype=np.float32)
    c["rope_cos"], c["rope_sin"] = _rope_tables()
    sel = np.zeros((32, 32, 128), np.float32)
    for e in range(32):
        sel[e, e, :] = 1.0
    c["sel"] = sel.reshape(32, 32 * 128).astype(ml_dtypes.bfloat16)
    for tag, Ls in (("m", L), ("c", LC)):
        zT, dec = _filter_consts(Ls)
        c[f"zT_{tag}"] = zT
        c[f"dec_{tag}"] = dec
        t = _dft_tables(Ls)
        for k in ("CF", "SF", "CI", "SI"):
            c[f"{k}_{tag}"] = t[k]
    _CACHE["c"] = c
    return c


def kernel(x, c, ctx, c_ctx, ada_w, ada_b, norm_mix, norm_ffn, w_in, q_norm, k_norm, rpb,
           short_w, short_b, flt_w1, flt_b1, flt_w2, flt_b2, flt_w3, hy_skip, w_br_a, w_br_b, w_out,
           w_group, b_group, w_router, b_router, moe_w1, moe_w3, moe_w2):
    f = lambda a: np.ascontiguousarray(np.asarray(a, dtype=np.float32))
    x, c, ctx, c_ctx = f(x), f(c), f(ctx), f(c_ctx)
    shared = dict(_host_consts())
    shared["ada_w"] = f(ada_w)
    shared["ada_bT"] = f(np.asarray(ada_b).reshape(DEPTH, 48, 128).transpose(0, 2, 1))
    shared["norm_mixT"] = f(np.asarray(norm_mix).reshape(DEPTH, 8, 128).transpose(0, 2, 1))
    shared["norm_ffnT"] = f(np.asarray(norm_ffn).reshape(DEPTH, 8, 128).transpose(0, 2, 1))
    shared["w_in"] = f(w_in)
    shared["q_norm"] = f(q_norm)
    shared["k_norm"] = f(k_norm)
    shared["biasT"] = _bias_tables(f(rpb)).reshape(DEPTH, 5, 128, 8 * 5 * 128)
    shared["short_wT"] = f(np.asarray(short_w).reshape(DEPTH, 3, 12, 128).transpose(0, 3, 2, 1))
    shared["short_bT"] = f(np.asarray(short_b).reshape(DEPTH, 12, 128).transpose(0, 2, 1))
    shared["flt_w1"] = f(flt_w1)
    shared["flt_b1"] = f(np.asarray(flt_b1).reshape(DEPTH, 64, 1))
    shared["flt_w2"] = f(flt_w2)
    shared["flt_b2"] = f(np.asarray(flt_b2).reshape(DEPTH, 64, 1))
    shared["flt_w3"] = f(flt_w3)
    shared["hy_skip"] = f(hy_skip)
    shared["w_br_a"] = f(w_br_a)
    shared["w_br_b"] = f(w_br_b)
    shared["w_out"] = f(w_out)
    shared["w_gr"] = f(np.concatenate([np.asarray(w_group), np.asarray(w_router)], axis=-1))
    shared["b_gr"] = f(np.concatenate([np.asarray(b_group), np.asarray(b_router)], axis=-1))
    shared["moe_w1"] = f(np.asarray(moe_w1).reshape(DEPTH, 32, D, 256))
    shared["moe_w3"] = f(np.asarray(moe_w3).reshape(DEPTH, 32, D, 256))
    shared["moe_w2"] = f(np.asarray(moe_w2).reshape(DEPTH, 32, 256, D))
    if "nc" not in _CACHE:
        _CACHE["nc"] = KB().build()
    nc = _CACHE["nc"]
    in_maps = []
    for i in range(NCORES):
        m = dict(shared)
        m["x"] = x[2 * i:2 * i + 2]
        m["ctx"] = ctx[2 * i:2 * i + 2]
        cT = np.empty((2, 128, 8, 2), np.float32)
        for bb in range(2):
            cT[bb, :, :, 0] = c[2 * i + bb].reshape(8, 128).T
            cT[bb, :, :, 1] = c_ctx.reshape(8, 128).T
        m["cT"] = cT
        in_maps.append(m)
    res = run_bass_kernel_spmd(nc, in_maps, core_ids=list(range(NCORES)))
    _CACHE["last"] = res
    out = np.concatenate([r["out"] for r in res.results], axis=0)
    return out.astype(np.float32)
```

```python
import os
import numpy as np
import ml_dtypes
import concourse.bass as bass
import concourse.mybir as mybir
from concourse.bass_utils import run_bass_kernel_spmd

F32 = mybir.dt.float32
BF16 = mybir.dt.bfloat16
I32 = mybir.dt.int32
AF = mybir.ActivationFunctionType
ALU = mybir.AluOpType
AX = mybir.AxisListType

SAME_ENGINE_SYNC = True
PE_T32 = os.environ.get('MK_PET32', '1') == '1'
NCORES = 8
D = 1024
L = 2048
LC = 256
NT = L + LC
DEPTH = 2
EPS = 1e-6
TB = [(0, 512, 0), (512, 512, 0), (1024, 512, 0), (1536, 512, 0), (2048, 256, 1)]
DBG_STAGE = os.environ.get("MK_STAGE", "")
DBG_COLS = 8192 if DBG_STAGE else 64


class Res:
    _n = 0

    def __init__(self, name=None):
        Res._n += 1
        self.name = (name or "r") + str(Res._n)
        self.writers = {}
        self.readers = {}
        self.slot = None


class Slot:
    def __init__(self, sem):
        self.sem = sem
        self.total = 0


class Op:
    __slots__ = ("fn", "deps", "signal", "dma_res", "dma_val")

    def __init__(self, fn, deps):
        self.fn = fn
        self.deps = deps
        self.signal = False
        self.dma_res = None
        self.dma_val = 0


class Prog:
    ENG = ("tensor", "vector", "scalar", "gpsimd", "sync")

    def __init__(self, nc):
        self.nc = nc
        self.ops = {e: [] for e in self.ENG}
        self.slots = []
        self.free_slots = []

    @staticmethod
    def _key(ref):
        return ref[1] if ref[0] == "e" else ("d", id(ref[1]))

    def _collect(self, eng, mykey, reads, writes, accs):
        deps = {}

        def add(ref, war=False):
            if ref[0] == "e" and ref[1] == eng and (eng == "tensor" or not SAME_ENGINE_SYNC or (war and eng == "vector")):
                return
            k = self._key(ref)
            if k not in deps or deps[k][2] < ref[2]:
                deps[k] = ref

        for r in reads:
            for ref in r.writers.values():
                add(ref)
        for w in writes:
            for ref in w.writers.values():
                add(ref)
            for ref in w.readers.values():
                add(ref, True)
        for w in accs:
            for k, ref in w.writers.items():
                if k != mykey:
                    add(ref)
            for ref in w.readers.values():
                add(ref, True)
        return list(deps.values())

    @staticmethod
    def _update(mykey, ref, reads, writes, accs):
        for w in writes:
            w.writers = {mykey: ref}
            w.readers = {}
        for w in accs:
            w.writers[mykey] = ref
        for r in reads:
            r.readers[mykey] = ref

    def op(self, eng, fn, reads=(), writes=(), accs=()):
        lst = self.ops[eng]
        deps = self._collect(eng, eng, reads, writes, accs)
        lst.append(Op(fn, deps))
        ref = ("e", eng, len(lst) - 1)
        self._update(eng, ref, reads, writes, accs)
        return ref

    def dma(self, out_ap, in_ap, owner, reads=(), writes=(), accs=(), eng="sync"):
        if owner.slot is None:
            if self.free_slots:
                owner.slot = self.free_slots.pop()
            else:
                owner.slot = Slot(self.nc.alloc_semaphore("dq%d" % len(self.slots)))
                self.slots.append(owner.slot)
        slot = owner.slot
        mykey = ("d", id(slot))
        deps = self._collect(eng, mykey, reads, writes, accs)
        slot.total += 16
        o = Op(lambda e: e.dma_start(out=out_ap, in_=in_ap), deps)
        o.dma_res = slot
        o.dma_val = slot.total
        self.ops[eng].append(o)
        ref = ("d", slot, slot.total)
        self._update(mykey, ref, reads, writes, accs)
        return ref

    def barrier(self):
        last = {}
        for e in self.ENG:
            if e != "sync" and self.ops[e]:
                last[e] = ("e", e, len(self.ops[e]) - 1)
        dmas = [("d", o, o.total) for o in self.slots if o.total > 0]
        for e in self.ENG:
            deps = [r for k, r in last.items() if k != e] + dmas
            self.ops[e].append(Op(None, deps))

    def build(self):
        nc = self.nc
        for e in self.ENG:
            for o in self.ops[e]:
                for d in o.deps:
                    if d[0] == "e":
                        self.ops[d[1]][d[2]].signal = True
        signum = {}
        for e in self.ENG:
            c = 0
            nums = []
            for o in self.ops[e]:
                if o.signal:
                    c += 1
                nums.append(c)
            signum[e] = nums
        esem = {e: nc.alloc_semaphore("e_" + e) for e in self.ENG if e != "sync"}

        def replay(ename):
            def run(engine):
                waited = {}
                for o in self.ops[ename]:
                    for d in o.deps:
                        if d[0] == "e":
                            sem = esem[d[1]]
                            val = signum[d[1]][d[2]]
                        else:
                            sem = d[1].sem
                            val = d[2]
                        sid = id(sem)
                        if waited.get(sid, 0) >= val:
                            continue
                        waited[sid] = val
                        engine.wait_ge(sem, val)
                    if o.fn is None:
                        if o.signal:
                            engine.nop().then_inc(esem[ename], 1)
                        continue
                    ins = o.fn(engine)
                    if o.dma_res is not None:
                        ins.then_inc(o.dma_res.sem, 16)
                    elif o.signal:
                        ins.then_inc(esem[ename], 1)
            return run

        with nc.Block() as block:
            block.tensor(replay("tensor"))
            block.vector(replay("vector"))
            block.scalar(replay("scalar"))
            block.gpsimd(replay("gpsimd"))
            block.sync(replay("sync"))


class T:
    def __init__(self, t, name):
        self.t = t
        self.r = Res(name)

    def __getitem__(self, k):
        return self.t[k]


def _dft_tables(Ls):
    N = 2 * Ls
    nt = Ls // 128
    s = np.arange(Ls, dtype=np.float64)
    w = 2.0 * np.pi * (np.arange(Ls, dtype=np.float64) + 0.5) / N
    ang = np.outer(s, w)
    out = {}
    for nm, M in (("C", np.cos(ang)), ("S", np.sin(ang))):
        F = M.reshape(nt, 128, nt, 128).transpose(2, 1, 0, 3)
        I = M.reshape(nt, 128, nt, 128).transpose(0, 3, 2, 1)
        out[nm + "F"] = np.ascontiguousarray(F).astype(ml_dtypes.bfloat16)
        out[nm + "I"] = np.ascontiguousarray(I).astype(ml_dtypes.bfloat16)
    return out


def _filter_consts(Ls):
    pos = np.arange(Ls, dtype=np.float32)
    t = pos / np.float32(max(Ls - 1, 1))
    w = (np.float32(2.0 * np.pi) * pos / np.float32(Ls)).astype(np.float32)
    f = np.linspace(1e-4, 15, 16, dtype=np.float32)
    arg = (f[None, :] * w[:, None]).astype(np.float32).astype(np.float64)
    z = np.concatenate([t[:, None].astype(np.float64), np.cos(arg), -np.sin(arg)], axis=-1)
    zT = np.ascontiguousarray(z.T).astype(np.float32)
    hmax = np.log(1e-2) / 0.3
    hmin = np.log(1e-2) / 1.5
    deltas = np.abs(np.linspace(hmin, hmax, 512, dtype=np.float32))
    dec = np.exp(-(t[:, None].astype(np.float32) * deltas[None, :]).astype(np.float32)).astype(np.float32)
    return zT, dec


def _rope_tables():
    pos = np.arange(L)
    rows, cols = pos // 64, pos % 64
    freqs = (100.0 ** (-np.arange(16, dtype=np.float32) / 16)).astype(np.float32)
    ar = (rows.astype(np.float32)[:, None] * freqs[None, :]).astype(np.float64)
    ac = (cols.astype(np.float32)[:, None] * freqs[None, :]).astype(np.float64)
    COS = np.concatenate([np.cos(ar), np.cos(ar), np.cos(ac), np.cos(ac)], axis=-1).astype(np.float32)
    SIN = np.concatenate([-np.sin(ar), np.sin(ar), -np.sin(ac), np.sin(ac)], axis=-1).astype(np.float32)
    return COS, SIN


ATT_CLS = []
for _i in range(16):
    if _i < 2:
        ATT_CLS.append((_i, [0, 1, 2, 3, 3]))
    elif _i < 14:
        ATT_CLS.append((2, [_i - 2, _i - 1, _i, _i + 1, _i + 2]))
    else:
        ATT_CLS.append((3 + (_i - 14), [12, 13, 14, 15, 15]))
_CLS_REP = {0: 0, 1: 1, 2: 2, 3: 14, 4: 15}
_CLS_VALID = {0: 4, 1: 4, 2: 5, 3: 4, 4: 4}


def _bias_tables(rpb):
    out = np.full((DEPTH, 5, 128, 8, 5, 128), -100.0, np.float32)
    qi = np.arange(128)
    ki = np.arange(128)
    for cls in range(5):
        i = _CLS_REP[cls]
        tiles = ATT_CLS[i][1]
        r = 2 * i + qi // 64
        col = qi % 64
        rs = np.clip(r - 4, 0, 24)
        ws = np.clip(col - 8, 0, 48)
        for c in range(_CLS_VALID[cls]):
            tok = tiles[c] * 128 + ki
            kr, kc = tok // 64, tok % 64
            inw = ((kr[:, None] >= rs[None, :]) & (kr[:, None] < rs[None, :] + 8) &
                   (kc[:, None] >= ws[None, :]) & (kc[:, None] < ws[None, :] + 16))
            dr = np.clip(kr[:, None] - r[None, :] + 7, 0, 14)
            dc = np.clip(kc[:, None] - col[None, :], -15, 15) + 15
            for l in range(DEPTH):
                g = rpb[l][:, dr, dc]
                g = np.where(inw[None], g, np.float32(-100.0))
                out[l, cls, :, :, c, :] = g.transpose(1, 0, 2)
    return out


class KB:
    def __init__(self):
        self.nc = bass.Bass("TRN2", target_bir_lowering=False)
        nc = self.nc
        self.P = Prog(nc)
        self.sb_lo = (nc.sbuf_base + 31) // 32 * 32
        self.sb_hi = nc.sbuf_top
        self.sp = self.sb_lo
        self.nalloc = 0
        self.live = []
        self.ps = [T(nc.alloc_psum_tensor(f"ps{i}", [128, 512], F32), f"ps{i}") for i in range(8)]
        self.ps_i = 0
        self.dram = {}

    def sb(self, name, shape, dt=F32):
        n = int(np.prod(shape[1:])) * (4 if dt in (F32, I32) else 2)
        n = (n + 31) // 32 * 32
        assert self.sp + n <= self.sb_hi, f"SBUF overflow allocating {name}: {self.sp + n - self.sb_lo}"
        self.nalloc += 1
        t = self.nc.alloc_sbuf_tensor_at(f"{name}_{self.nalloc}", list(shape), dt, offset=self.sp)
        tt_ = T(t, name)
        self.live.append((self.sp, tt_))
        self.sp += n
        return tt_

    def mark(self):
        return self.sp

    def release(self, m):
        self.P.barrier()
        keep = []
        for off, t in self.live:
            if off >= m:
                if t.r.slot is not None:
                    self.P.free_slots.append(t.r.slot)
                    t.r.slot = None
            else:
                keep.append((off, t))
        self.live = keep
        self.sp = m

    def psum(self):
        p = self.ps[self.ps_i % 6]
        self.ps_i += 1
        return p

    def din(self, name, shape, dt=F32):
        self.dram[name] = self.nc.dram_tensor(name, list(shape), dt, kind="ExternalInput").ap()
        return self.dram[name]

    def dscr(self, name, shape, dt=F32):
        return T(self.nc.dram_tensor(name, list(shape), dt).ap(), name)

    def mm(self, pt, pap, lhsT, rhs, start, stop, rd):
        self.P.op("tensor", lambda e: e.matmul(pap, lhsT=lhsT, rhs=rhs, start=start, stop=stop),
                  reads=rd, writes=[pt.r] if start else (), accs=() if start else [pt.r])

    def tr32(self, pt, pap, src, ident, rd, first):
        if PE_T32:
            self.P.op("tensor", lambda e: e.transpose(out=pap, in_=src, identity=ident),
                      reads=rd, writes=[pt.r] if first else (), accs=() if first else [pt.r])
        else:
            self.mm_new(pt, pap, src, ident, True, True, rd, first)

    def mm_new(self, pt, pap, lhsT, rhs, start, stop, rd, first):
        self.P.op("tensor", lambda e: e.matmul(pap, lhsT=lhsT, rhs=rhs, start=start, stop=stop),
                  reads=rd, writes=[pt.r] if first else (), accs=() if first else [pt.r])

    def act(self, out, in_, func, rd, wr=(), acc=(), **kw):
        self.P.op("scalar", lambda e: e.activation(out=out, in_=in_, func=func, **kw), reads=rd, writes=wr, accs=acc)

    def tt(self, eng, out, in0, in1, op, rd, wr=(), acc=()):
        self.P.op(eng, lambda e: e.tensor_tensor(out=out, in0=in0, in1=in1, op=op), reads=rd, writes=wr, accs=acc)

    def ts(self, eng, out, in0, s1, s2, op0, op1, rd, wr=(), acc=()):
        if s2 is None:
            self.P.op(eng, lambda e: e.tensor_scalar(out=out, in0=in0, scalar1=s1, scalar2=None, op0=op0), reads=rd, writes=wr, accs=acc)
        else:
            self.P.op(eng, lambda e: e.tensor_scalar(out=out, in0=in0, scalar1=s1, scalar2=s2, op0=op0, op1=op1), reads=rd, writes=wr, accs=acc)

    def stt(self, eng, out, in0, scalar, in1, op0, op1, rd, wr=(), acc=()):
        self.P.op(eng, lambda e: e.scalar_tensor_tensor(out=out, in0=in0, scalar=scalar, in1=in1, op0=op0, op1=op1),
                  reads=rd, writes=wr, accs=acc)

    def cp(self, eng, out, in_, rd, wr=(), acc=()):
        if eng == "scalar":
            self.P.op(eng, lambda e: e.copy(out=out, in_=in_), reads=rd, writes=wr, accs=acc)
        else:
            self.P.op(eng, lambda e: e.tensor_copy(out=out, in_=in_), reads=rd, writes=wr, accs=acc)

    def memset(self, eng, ap, val, wr=(), acc=()):
        self.P.op(eng, lambda e: e.memset(ap, val), writes=wr, accs=acc)

    def recip(self, out, in_, rd, wr=(), acc=()):
        self.P.op("vector", lambda e: e.reciprocal(out=out, in_=in_), reads=rd, writes=wr, accs=acc)

    def red(self, out, in_, op, rd, wr=(), acc=()):
        if op == "max":
            self.P.op("vector", lambda e: e.reduce_max(out=out, in_=in_, axis=AX.X), reads=rd, writes=wr, accs=acc)
        else:
            self.P.op("vector", lambda e: e.reduce_sum(out=out, in_=in_, axis=AX.X), reads=rd, writes=wr, accs=acc)

    def dma(self, out_ap, in_ap, owner, rd=(), wr=(), acc=()):
        self.P.dma(out_ap, in_ap, owner, reads=rd, writes=wr, accs=acc)

    def build(self):
        nc, P = self.nc, self.P
        din, sb = self.din, self.sb
        x_d = din("x", [2, L, D])
        ctx_d = din("ctx", [2, LC, D])
        cT_d = din("cT", [2, 128, 8, 2])
        ada_w = din("ada_w", [DEPTH, D, 6 * D])
        ada_bT = din("ada_bT", [DEPTH, 128, 48])
        nmT_d = din("norm_mixT", [DEPTH, 128, 8])
        nfT_d = din("norm_ffnT", [DEPTH, 128, 8])
        w_in = din("w_in", [DEPTH, D, 5120])
        qn_d = din("q_norm", [DEPTH, 64])
        kn_d = din("k_norm", [DEPTH, 64])
        biasT_d = din("biasT", [DEPTH, 5, 128, 8 * 5 * 128])
        swT_d = din("short_wT", [DEPTH, 128, 12, 3])
        sbT_d = din("short_bT", [DEPTH, 128, 12])
        fw1_d = din("flt_w1", [DEPTH, 33, 64])
        fb1_d = din("flt_b1", [DEPTH, 64, 1])
        fw2_d = din("flt_w2", [DEPTH, 64, 64])
        fb2_d = din("flt_b2", [DEPTH, 64, 1])
        fw3_d = din("flt_w3", [DEPTH, 64, 2048])
        skip_d = din("hy_skip", [DEPTH, 2, 512])
        wbra_d = din("w_br_a", [DEPTH, 512, D])
        wbrb_d = din("w_br_b", [DEPTH, 512, D])
        wout_d = din("w_out", [DEPTH, D, D])
        wgr_d = din("w_gr", [DEPTH, D, 36])
        bgr_d = din("b_gr", [DEPTH, 36])
        mw1_d = din("moe_w1", [DEPTH, 32, D, 256])
        mw3_d = din("moe_w3", [DEPTH, 32, D, 256])
        mw2_d = din("moe_w2", [DEPTH, 32, 256, D])
        ident_d = din("ident", [128, 128])
        cos_d = din("rope_cos", [L, 64])
        sin_d = din("rope_sin", [L, 64])
        sel_d = din("sel", [32, 32 * 128], BF16)
        tabs = {}
        for tag, Ls in (("m", L), ("c", LC)):
            nt = Ls // 128
            tabs[tag] = dict(
                L=Ls, nt=nt,
                zT=din(f"zT_{tag}", [33, Ls]), dec=din(f"dec_{tag}", [Ls, 512]),
                CF=din(f"CF_{tag}", [nt, 128, nt, 128], BF16), SF=din(f"SF_{tag}", [nt, 128, nt, 128], BF16),
                CI=din(f"CI_{tag}", [nt, 128, nt, 128], BF16), SI=din(f"SI_{tag}", [nt, 128, nt, 128], BF16),
                KF=[self.dscr(f"KF_{tag}{l}", [nt, 128, 2, 1024]) for l in range(DEPTH)],
            )
        out_d = self.nc.dram_tensor("out", [2, L, D], F32, kind="ExternalOutput").ap()
        out_r = Res("out")
        dbg_d = self.nc.dram_tensor("dbg", [128, DBG_COLS], F32, kind="ExternalOutput").ap()
        xscr = self.dscr("xscr", [128, 8, NT])
        escr = [[self.dscr(f"escr{l}_{c}", [128, 8 * 5 * 128], BF16) for c in range(5)] for l in range(DEPTH)]
        mixT = self.dscr("mixT", [128, 8, NT], BF16)
        gscr = self.dscr("gscr", [128, 16, NT], BF16)

        identf = sb("identf", [128, 128])
        identb = sb("identb", [128, 128], BF16)
        onesf = sb("onesf", [128, 128])
        onesb = sb("onesb", [128, 128], BF16)
        cst = sb("cst", [128, 4])
        nmT = sb("nmT", [128, DEPTH, 8])
        nfT = sb("nfT", [128, DEPTH, 8])
        scl = sb("scl", [128, 2, 6, 8])
        gainq = sb("gainq", [128, 8, 64])
        gaink = sb("gaink", [128, 8, 64])
        self.dma(identf[:], ident_d, identf.r, wr=[identf.r])
        self.cp("vector", identb[:], identf[:], [identf.r], wr=[identb.r])
        self.memset("vector", onesf[:], 1.0, wr=[onesf.r])
        self.memset("vector", onesb[:], 1.0, wr=[onesb.r])
        self.memset("vector", cst[:, 0:1], float(-np.pi), wr=[cst.r])
        self.memset("vector", cst[:, 1:2], EPS, acc=[cst.r])
        self.memset("vector", cst[:, 2:3], 0.0, acc=[cst.r])
        self.dma(nmT[:], nmT_d.rearrange("l p j -> p l j"), nmT.r, wr=[nmT.r])
        self.dma(nfT[:], nfT_d.rearrange("l p j -> p l j"), nfT.r, wr=[nfT.r])
        base_mark = self.mark()
        dbg_done = [False]

        def dump(src_ap, ncols, rd):
            st = sb("dbgst", [128, ncols])
            self.cp("vector", st[:], src_ap, rd, wr=[st.r])
            self.dma(dbg_d[:, 0:ncols], st[:], st.r, rd=[st.r], wr=[out_r])
            dbg_done[0] = True

        def phase_emask(l):
            m = self.mark()
            stg = [sb("est", [128, 5120]) for _ in range(2)]
            eb = [sb("eb", [128, 5120], BF16) for _ in range(2)]
            for c in range(5):
                s, b = stg[c % 2], eb[c % 2]
                self.dma(s[:], biasT_d[l, c], s.r, wr=[s.r])
                self.act(b[:], s[:], AF.Exp, [s.r], wr=[b.r])
                self.dma(escr[l][c][:], b[:], b.r, rd=[b.r], wr=[escr[l][c].r])
            self.release(m)

        def sin_rr(dst_ap, ps_t, ps_ap, bcol, n, tmps, wr, acc):
            u, ki, fr = tmps
            self.act(u[0:64, 0:n], ps_ap, AF.Identity, [ps_t.r, bcol.r], wr=[u.r], bias=bcol[0:64, 0:1], scale=float(1.0 / (2 * np.pi)))
            self.cp("vector", ki[0:64, 0:n], u[0:64, 0:n], [u.r], wr=[ki.r])
            self.tt("vector", fr[0:64, 0:n], u[0:64, 0:n], ki[0:64, 0:n], ALU.subtract, [u.r, ki.r], wr=[fr.r])
            self.stt("vector", u[0:64, 0:n], fr[0:64, 0:n], 0.0, fr[0:64, 0:n], ALU.is_lt, ALU.add, [fr.r], wr=[u.r])
            self.act(dst_ap, u[0:64, 0:n], AF.Sin, [u.r, cst.r], wr=wr, acc=acc, bias=cst[0:64, 0:1], scale=float(2 * np.pi))

        def phase_filter(l, tb):
            Ls, nt = tb["L"], tb["nt"]
            N = 2 * Ls
            m = self.mark()
            zT = sb("zT", [33, Ls]); w1 = sb("fw1", [33, 64]); w2 = sb("fw2", [64, 64]); w3 = sb("fw3", [64, 2048])
            b1 = sb("fb1", [64, 1]); b2 = sb("fb2", [64, 1])
            h1 = sb("h1", [64, Ls]); h2 = sb("h2", [64, Ls])
            tmps = (sb("u", [64, 512]), sb("ki", [64, 512], I32), sb("fr", [64, 512]))
            hd_ = [sb("hd", [128, 4, 512]) for _ in range(2)]; ab_ = [sb("ab", [128, 4, 512]) for _ in range(2)]; asum_ = [sb("asum", [128, 2, 512]) for _ in range(2)]
            dec = [sb("dec", [128, 512]) for _ in range(2)]
            pm = sb("pm", [128, nt, 2, 1024], BF16)
            rn = sb("rn", [128, 2, 512])
            tc_ = [sb("tC", [128, nt, 128], BF16) for _ in range(2)]
            ts_ = [sb("tS", [128, nt, 128], BF16) for _ in range(2)]
            kst = [sb("kst", [128, 2, 1024]) for _ in range(2)]
            self.dma(zT[:], tb["zT"], zT.r, wr=[zT.r])
            self.dma(w1[:], fw1_d[l], w1.r, wr=[w1.r])
            self.dma(w2[:], fw2_d[l], w2.r, wr=[w2.r])
            self.dma(w3[:], fw3_d[l], w3.r, wr=[w3.r])
            self.dma(b1[:], fb1_d[l], b1.r, wr=[b1.r])
            self.dma(b2[:], fb2_d[l], b2.r, wr=[b2.r])
            for b in (b1, b2):
                self.ts("vector", b[:], b[:], float(1.0 / (2 * np.pi)), 8.5, ALU.mult, ALU.add, [b.r], wr=[b.r])
            nb = (Ls + 511) // 512
            for j in range(nb):
                n = min(512, Ls - j * 512)
                p = self.psum()
                self.mm(p, p[0:64, 0:n], w1[:], zT[:, j * 512:j * 512 + n], True, True, [w1.r, zT.r])
                sin_rr(h1[:, j * 512:j * 512 + n], p, p[0:64, 0:n], b1, n, tmps, (), [h1.r])
            for j in range(nb):
                n = min(512, Ls - j * 512)
                p = self.psum()
                self.mm(p, p[0:64, 0:n], w2[:], h1[:, j * 512:j * 512 + n], True, True, [w2.r, h1.r])
                sin_rr(h2[:, j * 512:j * 512 + n], p, p[0:64, 0:n], b2, n, tmps, (), [h2.r])
            pn = [self.ps[6], self.ps[7]]

            def f_A(lt):
                dc_ = dec[lt % 2]
                hd, ab, asum = hd_[lt % 2], ab_[lt % 2], asum_[lt % 2]
                self.dma(dc_[:], tb["dec"][lt * 128:(lt + 1) * 128, :], dc_.r, wr=[dc_.r])
                for cg in range(4):
                    p = self.psum()
                    self.mm(p, p[:, :], h2[:, lt * 128:(lt + 1) * 128], w3[:, cg * 512:(cg + 1) * 512], True, True, [h2.r, w3.r])
                    self.tt("vector", hd[:, cg, :], p[:, :], dc_[:], ALU.mult, [p.r, dc_.r], wr=[hd.r] if cg == 0 else (), acc=() if cg == 0 else [hd.r])
                if lt == 0:
                    self.memset("vector", hd[0:1, 2:4, :], 0.0, acc=[hd.r])
                self.act(ab[:], hd[:], AF.Abs, [hd.r], wr=[ab.r])
                self.tt("gpsimd", asum[:], ab[:, 0:2, :], ab[:, 2:4, :], ALU.add, [ab.r], wr=[asum.r])
                self.tt("vector", pm[:, lt, 0, :].rearrange("p (o c) -> p o c", o=2), hd[:, 0:2, :], hd[:, 2:4, :], ALU.add, [hd.r], acc=[pm.r])
                self.tt("gpsimd", pm[:, lt, 1, :].rearrange("p (o c) -> p o c", o=2), hd[:, 2:4, :], hd[:, 0:2, :], ALU.subtract, [hd.r], acc=[pm.r])

            def f_B(lt):
                asum = asum_[lt % 2]
                for o in range(2):
                    self.mm(pn[o], pn[o][:, :], onesf[:], asum[:, o, :], lt == 0, lt == nt - 1, [onesf.r, asum.r])

            for lt in range(nt + 1):
                if lt < nt:
                    f_A(lt)
                if lt >= 1:
                    f_B(lt - 1)
            for o in range(2):
                self.ts("vector", rn[:, o, :], pn[o][:, :], EPS, float(N / 2.0), ALU.add, ALU.mult, [pn[o].r], wr=[rn.r] if o == 0 else (), acc=() if o == 0 else [rn.r])
            self.recip(rn[:], rn[:], [rn.r], wr=[rn.r])
            for fc in range(nt):
                tC, tS, ks = tc_[fc % 2], ts_[fc % 2], kst[fc % 2]
                self.dma(tC[:], tb["CF"][fc], tC.r, wr=[tC.r])
                self.dma(tS[:], tb["SF"][fc], tS.r, wr=[tS.r])
                for o in range(2):
                    pr, pi = self.psum(), self.psum()
                    for lt in range(nt):
                        self.mm(pr, pr[:, :], tC[:, lt, :], pm[:, lt, 0, o * 512:(o + 1) * 512], lt == 0, lt == nt - 1, [tC.r, pm.r])
                    for lt in range(nt):
                        self.mm(pi, pi[:, :], tS[:, lt, :], pm[:, lt, 1, o * 512:(o + 1) * 512], lt == 0, lt == nt - 1, [tS.r, pm.r])
                    first = (o == 0)
                    self.tt("vector", ks[:, 0, o * 512:(o + 1) * 512], pr[:, :], rn[:, o, :], ALU.mult, [pr.r, rn.r], wr=[ks.r] if first else (), acc=() if first else [ks.r])
                    self.tt("vector", ks[:, 1, o * 512:(o + 1) * 512], pi[:, :], rn[:, o, :], ALU.mult, [pi.r, rn.r], acc=[ks.r])
                self.dma(tb["KF"][l][fc], ks[:], ks.r, rd=[ks.r], acc=[tb["KF"][l].r])
            self.release(m)

        def phase_load(b):
            m = self.mark()
            xT = sb("xT", [128, 8, NT])
            stg = [sb("xst", [128, D]) for _ in range(2)]
            for t in range(18):
                s = stg[t % 2]
                src = x_d[b, t * 128:(t + 1) * 128, :] if t < 16 else ctx_d[b, (t - 16) * 128:(t - 15) * 128, :]
                self.dma(s[:], src, s.r, wr=[s.r])
                for half in range(2):
                    p = self.psum()
                    for j in range(4):
                        dc = half * 4 + j
                        self.tr32(p, p[:, j * 128:(j + 1) * 128], s[:, dc * 128:(dc + 1) * 128], identf[:], [s.r, identf.r], j == 0)
                    self.cp("scalar" if half else "vector", xT[:, half * 4:half * 4 + 4, t * 128:(t + 1) * 128],
                            p[:, :].rearrange("p (j c) -> p j c", j=4), [p.r], acc=[xT.r])
            self.dma(xscr[:], xT[:], xT.r, rd=[xT.r], wr=[xscr.r])
            self.release(m)

        def phase_ada(l, b):
            m = self.mark()
            craw = sb("craw", [128, 8, 2]); sT = sb("sT", [128, 8, 2])
            wst = [sb("adaw", [128, 8, 512]) for _ in range(2)]
            modT = sb("modT", [128, 48, 2]); adab = sb("adab", [128, 48]); tmp = sb("atmp", [128, 8])
            self.dma(craw[:], cT_d[b], craw.r, wr=[craw.r])
            self.dma(adab[:], ada_bT[l], adab.r, wr=[adab.r])
            self.act(sT[:], craw[:], AF.Silu, [craw.r], wr=[sT.r])
            pA = self.ps[6]
            for cg in range(12):
                w = wst[cg % 2]
                self.dma(w[:], ada_w[l][:, cg * 512:(cg + 1) * 512].rearrange("(dc p) c -> p dc c", p=128), w.r, wr=[w.r])
                for j4 in range(4):
                    j = cg * 4 + j4
                    for dc in range(8):
                        self.mm_new(pA, pA[:, j * 2:(j + 1) * 2], w[:, dc, j4 * 128:(j4 + 1) * 128], sT[:, dc, :], dc == 0, dc == 7, [w.r, sT.r], j == 0 and dc == 0)
            self.tt("vector", modT[:], pA[:, 0:96].rearrange("p (j s) -> p j s", s=2), adab[:].unsqueeze(2).to_broadcast([128, 48, 2]), ALU.add,
                    [pA.r, adab.r], wr=[modT.r])
            for s in range(2):
                first = (s == 0)
                for kind, (seg, nrm) in enumerate(((1, nmT), (0, None), (2, None), (4, nfT), (3, None), (5, None))):
                    src = modT[:, seg * 8:(seg + 1) * 8, s]
                    w_ = dict(wr=[scl.r]) if (first and kind == 0) else dict(acc=[scl.r])
                    if nrm is None:
                        self.cp("vector", scl[:, s, kind, :], src, [modT.r], **w_)
                    else:
                        self.ts("vector", tmp[:], src, 1.0, None, ALU.add, None, [modT.r], wr=[tmp.r])
                        self.tt("vector", scl[:, s, kind, :], tmp[:], nrm[:, l, :], ALU.mult, [tmp.r, nrm.r], **w_)
            self.dma(gainq[:], qn_d[l].partition_broadcast(128).unsqueeze(1).to_broadcast([128, 8, 64]), gainq.r, wr=[gainq.r])
            self.dma(gaink[:], kn_d[l].partition_broadcast(128).unsqueeze(1).to_broadcast([128, 8, 64]), gaink.r, wr=[gaink.r])
            self.ts("vector", gainq[:], gainq[:], 0.125, None, ALU.mult, None, [gainq.r], wr=[gainq.r])
            self.release(m)

        def norm_blocks(xT, outT, kindA, kindB, blocks, hook=None, f32buf=None):
            sq = [sb("sq", [128, 512], BF16) for _ in range(2)]
            rstd = sb("rstd", [128, 512])
            tmp = [sb("ntmp", [128, 512]) for _ in range(2)]
            pending = None
            for bi_, (t0, n, s) in enumerate(blocks):
                fb = f32buf[bi_ % 2] if f32buf is not None else None
                pS = self.psum()
                for dc in range(8):
                    q = sq[dc % 2]
                    self.act(q[:, 0:n], xT[:, dc, t0:t0 + n], AF.Square, [xT.r], wr=[q.r])
                    self.mm(pS, pS[:, 0:n], onesb[:], q[:, 0:n], dc == 0, dc == 7, [onesb.r, q.r])
                self.act(rstd[:, 0:n], pS[:, 0:n], AF.Sqrt, [pS.r, cst.r], wr=[rstd.r], scale=1.0 / D, bias=cst[:, 1:2])
                self.recip(rstd[:, 0:n], rstd[:, 0:n], [rstd.r], wr=[rstd.r])
                for dc in range(8):
                    tm = tmp[dc % 2]
                    self.stt("vector", tm[:, 0:n], xT[:, dc, t0:t0 + n], scl[:, s, kindA, dc:dc + 1], rstd[:, 0:n], ALU.mult, ALU.mult,
                             [xT.r, scl.r, rstd.r], wr=[tm.r])
                    if f32buf is None:
                        self.act(outT[:, dc, t0:t0 + n], tm[:, 0:n], AF.Identity, [tm.r, scl.r], acc=[outT.r], bias=scl[:, s, kindB, dc:dc + 1], scale=1.0)
                    else:
                        self.act(outT[:, dc, t0:t0 + n], tm[:, 0:n], AF.Identity, [tm.r, scl.r], acc=[outT.r], bias=scl[:, s, kindB, dc:dc + 1], scale=1.0)
                        self.ts("vector", fb[:, dc, 0:n], tm[:, 0:n], scl[:, s, kindB, dc:dc + 1], None, ALU.add, None, [tm.r, scl.r],
                                wr=[fb.r] if dc == 0 else (), acc=() if dc == 0 else [fb.r])
                if hook is not None:
                    if pending is not None:
                        hook(*pending)
                    pending = (t0, n, s, fb)
            if hook is not None and pending is not None:
                hook(*pending)

        def load_w_cols(dst, stg, src2d, c0, ncol):
            self.dma(stg[:, :, 0:ncol], src2d[:, c0:c0 + ncol].rearrange("(dc p) c -> p dc c", p=128), stg.r, wr=[stg.r])
            self.cp("gpsimd", dst[:, :, 0:ncol], stg[:, :, 0:ncol], [stg.r], wr=[dst.r])

        def transpose4(src_fn, nsrc, dst_ap, dst_t, rd, eng):
            p = self.psum()
            for j in range(nsrc):
                self.mm_new(p, p[:, j * 128:(j + 1) * 128], src_fn(j), identb[:], True, True, rd + [identb.r], j == 0)
            self.cp(eng, dst_ap, p[:, 0:nsrc * 128].rearrange("p (j c) -> p j c", j=nsrc), [p.r], acc=[dst_t.r])

        def layer_body(b, l, last):
            ntile = 16 if last else 18
            blocks_all = TB
            blocks_upd = TB[:4] if last else TB
            m0 = self.mark()
            phase_ada(l, b)
            hT = sb("hT", [128, 8, NT], BF16)
            mB = self.mark()
            xT = sb("xT", [128, 8, NT])
            self.dma(xT[:], xscr[:], xT.r, rd=[xscr.r], wr=[xT.r])
            norm_blocks(xT, hT, 0, 1, blocks_all)
            self.release(mB)
            if DBG_STAGE == "B":
                dump(hT[:, 0:2, 0:2304].rearrange("p a b -> p (a b)"), 4608, [hT.r]); return False
            qrT = sb("qrT", [128, 4, L], BF16); qpT = sb("qpT", [128, 4, NT], BF16); kT = sb("kT", [128, 4, NT], BF16)
            vaug = sb("vaug", [128, 18, 8, 65], BF16)
            mC = self.mark()
            wst = sb("wst", [128, 8, 512]); wbf = [sb("wbf", [128, 8, 512], BF16) for _ in range(2)]
            ropec = sb("ropec", [128, 16, 64]); ropes = sb("ropes", [128, 16, 64])
            self.dma(ropec[:], cos_d.rearrange("(t p) d -> p t d", p=128), ropec.r, wr=[ropec.r])
            self.dma(ropes[:], sin_d.rearrange("(t p) d -> p t d", p=128), ropes.r, wr=[ropes.r])
            qraw_ = [sb("qraw", [128, 512]) for _ in range(4)]
            sqt_ = [sb("sqt", [128, 512]) for _ in range(2)]; ssq_ = [sb("ssq", [128, 8]) for _ in range(3)]
            qn_ = [sb("qn", [128, 512]) for _ in range(2)]; qg_ = [sb("qg", [128, 512]) for _ in range(2)]
            t1_ = [sb("t1", [128, 512]) for _ in range(2)]; t2_ = [sb("t2", [128, 512]) for _ in range(2)]
            qpb_ = [sb("qpb", [128, 512], BF16) for _ in range(3)]; qrb_ = [sb("qrb", [128, 512], BF16) for _ in range(2)]
            self.memset("vector", vaug[:, :, :, 64:65], 1.0, wr=[vaug.r])
            v3 = lambda ap: ap.rearrange("p (h d) -> p h d", d=64)
            v5 = lambda ap: ap.rearrange("p (h r a d) -> p h r a d", h=8, r=2, a=2, d=16)
            items = [(0, t) for t in range(ntile)] + [(1, t) for t in range(18)]
            st_ = {}

            plain = lambda i: items[i][0] == 0 or items[i][1] >= 16
            rope = lambda i: items[i][1] < 16

            def s0(i):
                g, t = items[i]
                wb = wbf[g]
                if t == 0:
                    load_w_cols(wb, wst, w_in[l], g * 512, 512)
                p = self.psum()
                for dc in range(8):
                    self.mm(p, p[:, :], hT[:, dc, t * 128:(t + 1) * 128], wb[:, dc, :], dc == 0, dc == 7, [hT.r, wb.r])
                sqt, qraw = sqt_[i % 2], qraw_[i % 4]
                self.act(sqt[:], p[:, :], AF.Square, [p.r], wr=[sqt.r])
                self.cp("scalar", qraw[:], p[:, :], [p.r], wr=[qraw.r])

            def s1(i):
                sqt, ssq = sqt_[i % 2], ssq_[i % 3]
                self.red(ssq[:], v3(sqt[:]), "sum", [sqt.r], wr=[ssq.r])

            def s2(i):
                ssq = ssq_[i % 3]
                self.act(ssq[:], ssq[:], AF.Sqrt, [ssq.r, cst.r], wr=[ssq.r], scale=1.0 / 64, bias=cst[:, 1:2])

            def s3(i):
                ssq, qraw, qn = ssq_[i % 3], qraw_[i % 4], qn_[i % 2]
                self.recip(ssq[:], ssq[:], [ssq.r], wr=[ssq.r])
                self.tt("vector", v3(qn[:]), v3(qraw[:]), ssq[:].unsqueeze(2).to_broadcast([128, 8, 64]), ALU.mult, [qraw.r, ssq.r], wr=[qn.r])

            def s4(i):
                g, t = items[i]
                gain = gainq if g == 0 else gaink
                qn, qg = qn_[i % 2], qg_[i % 2]
                self.tt("gpsimd", v3(qg[:]), v3(qn[:]), gain[:], ALU.mult, [qn.r, gain.r], wr=[qg.r])

            def s5(i):
                g, t = items[i]
                qg, t1, t2, qpb = qg_[i % 2], t1_[i % 2], t2_[i % 2], qpb_[i % 3]
                if plain(i):
                    self.cp("scalar", qpb[:], qg[:], [qg.r], wr=[qpb.r])
                if rope(i):
                    self.tt("vector", v3(t1[:]), v3(qg[:]), ropec[:, t, :].unsqueeze(1).to_broadcast([128, 8, 64]), ALU.mult, [qg.r, ropec.r], wr=[t1.r])
                    sv = ropes[:, t, :].rearrange("p (r a d) -> p r a d", r=2, a=2, d=16)
                    self.tt("gpsimd", v5(t2[:])[:, :, :, 0, :], v5(qg[:])[:, :, :, 1, :], sv[:, :, 0, :].unsqueeze(1).to_broadcast([128, 8, 2, 16]), ALU.mult,
                            [qg.r, ropes.r], wr=[t2.r])
                    self.tt("gpsimd", v5(t2[:])[:, :, :, 1, :], v5(qg[:])[:, :, :, 0, :], sv[:, :, 1, :].unsqueeze(1).to_broadcast([128, 8, 2, 16]), ALU.mult,
                            [qg.r, ropes.r], acc=[t2.r])

            def s6(i):
                if rope(i):
                    t1, t2, qrb = t1_[i % 2], t2_[i % 2], qrb_[i % 2]
                    self.tt("vector", qrb[:], t1[:], t2[:], ALU.add, [t1.r, t2.r], wr=[qrb.r])

            def s7(i):
                g, t = items[i]
                outs = []
                if plain(i):
                    qpb = qpb_[i % 3]
                    p = self.psum()
                    for j in range(4):
                        self.mm_new(p, p[:, j * 128:(j + 1) * 128], qpb[:, j * 128:(j + 1) * 128], identb[:], True, True, [qpb.r, identb.r], j == 0)
                    outs.append((p, qpT if g == 0 else kT, "scalar"))
                if rope(i):
                    qrb = qrb_[i % 2]
                    p = self.psum()
                    for j in range(4):
                        self.mm_new(p, p[:, j * 128:(j + 1) * 128], qrb[:, j * 128:(j + 1) * 128], identb[:], True, True, [qrb.r, identb.r], j == 0)
                    outs.append((p, qrT if g == 0 else kT, "vector"))
                st_[i] = outs

            def s8(i):
                g, t = items[i]
                for (p, dstT, eng) in st_.pop(i):
                    self.cp(eng, dstT[:, :, t * 128:(t + 1) * 128], p[:, :].rearrange("p (j c) -> p j c", j=4), [p.r], acc=[dstT.r])

            stages = [s0, s1, s2, s3, s4, s5, s6, s7, s8]
            ni = len(items)
            for step in range(ni + len(stages) - 1):
                for k, fn in enumerate(stages):
                    i = step - k
                    if 0 <= i < ni:
                        fn(i)
            wb = wbf[0]
            load_w_cols(wb, wst, w_in[l], 2 * 512, 512)
            for t in range(18):
                p = self.psum()
                for dc in range(8):
                    self.mm(p, p[:, :], hT[:, dc, t * 128:(t + 1) * 128], wb[:, dc, :], dc == 0, dc == 7, [hT.r, wb.r])
                self.cp("scalar" if t % 2 else "vector", vaug[:, t, :, 0:64], v3(p[:, :]), [p.r], acc=[vaug.r])
            self.release(mC)
            if DBG_STAGE == "C1":
                dump(qrT[:, 0, 0:2048], 2048, [qrT.r]); return False
            emask = sb("emask", [128, 8, 5, 128], BF16)
            PT = [sb("PT", [128, 7, 128], BF16) for _ in range(4)]
            ao = sb("ao", [128, 8, 64], BF16); rec = sb("rec", [128, 8, 1])
            ast = [sb("ast", [128, 4, 128], BF16) for _ in range(2)]
            att = dict(cls=None, hc=0)
            pO = [self.ps[6], self.ps[7]]

            def att_S(i, h):
                main = i < 16
                if main:
                    cls, ktiles = ATT_CLS[i]
                    if h == 0 and cls != att["cls"]:
                        self.dma(emask[:].rearrange("p h c q -> p (h c q)"), escr[l][cls][:], emask.r, rd=[escr[l][cls].r], wr=[emask.r])
                        att["cls"] = cls
                pr_, hs = h // 2, (h % 2) * 64
                pt = PT[att["hc"] % 4]
                att["hc"] += 1
                qcols = slice(i * 128, (i + 1) * 128)
                pB = self.psum()
                if main:
                    pA = self.psum()
                    for c in range(4):
                        kt = ktiles[c]
                        self.mm_new(pA, pA[:, c * 128:(c + 1) * 128], kT[hs:hs + 64, pr_, kt * 128:(kt + 1) * 128], qrT[hs:hs + 64, pr_, qcols], True, True,
                                    [kT.r, qrT.r], c == 0)
                    kt = ktiles[4]
                    self.mm_new(pB, pB[:, 0:128], kT[hs:hs + 64, pr_, kt * 128:(kt + 1) * 128], qrT[hs:hs + 64, pr_, qcols], True, True, [kT.r, qrT.r], True)
                    for c in range(2):
                        self.mm_new(pB, pB[:, 128 + c * 128:256 + c * 128], kT[hs:hs + 64, pr_, L + c * 128:L + (c + 1) * 128], qpT[hs:hs + 64, pr_, qcols], True, True,
                                    [kT.r, qpT.r], False)
                    self.act(pt[:, 0:4, :], pA[:, :].rearrange("p (c q) -> p c q", c=4), AF.Exp, [pA.r], wr=[pt.r])
                    self.act(pt[:, 4:7, :], pB[:, 0:384].rearrange("p (c q) -> p c q", c=3), AF.Exp, [pB.r], acc=[pt.r])
                    self.tt("vector", pt[:, 0:5, :], pt[:, 0:5, :], emask[:, h, :, :], ALU.mult, [pt.r, emask.r], wr=[pt.r])
                    return pt, list(ktiles) + [16, 17], 7
                for c in range(2):
                    self.mm_new(pB, pB[:, c * 128:(c + 1) * 128], kT[hs:hs + 64, pr_, L + c * 128:L + (c + 1) * 128], qpT[hs:hs + 64, pr_, qcols], True, True,
                                [kT.r, qpT.r], c == 0)
                self.act(pt[:, 0:2, :], pB[:, 0:256].rearrange("p (c q) -> p c q", c=2), AF.Exp, [pB.r], wr=[pt.r])
                return pt, [16, 17], 2

            def att_PV(i, h, st):
                pt, vt, nch = st
                po = pO[h // 4]
                oc = (h % 4) * 65
                for c in range(nch):
                    self.mm_new(po, po[:, oc:oc + 65], pt[:, c, :], vaug[:, vt[c], h, :], c == 0, c == nch - 1, [pt.r, vaug.r], (h % 4 == 0) and c == 0)
                if h % 4 == 3:
                    pv = po[:, 0:260].rearrange("p (h e) -> p h e", e=65)
                    hh = (h // 4) * 4
                    self.recip(rec[:, hh:hh + 4, :], pv[:, :, 64:65], [po.r], acc=[rec.r])
                    self.tt("vector", ao[:, hh:hh + 4, :], pv[:, :, 0:64], rec[:, hh:hh + 4, :].to_broadcast([128, 4, 64]), ALU.mult, [po.r, rec.r], acc=[ao.r])
                if h == 7:
                    a_ = ast[i % 2]
                    aof = ao[:].rearrange("p h d -> p (h d)")
                    p = self.psum()
                    for j in range(4):
                        self.mm_new(p, p[:, j * 128:(j + 1) * 128], aof[:, j * 128:(j + 1) * 128], identb[:], True, True, [ao.r, identb.r], j == 0)
                    self.cp("scalar", a_[:], p[:, :].rearrange("p (j c) -> p j c", j=4), [p.r], wr=[a_.r])
                    self.dma(mixT[:, 0:4, i * 128:(i + 1) * 128], a_[:], a_.r, rd=[a_.r], acc=[mixT.r])

            pend = []
            for i in range(ntile):
                for h in range(8):
                    st = att_S(i, h)
                    pend.append((i, h, st))
                    if len(pend) > 2:
                        att_PV(*pend.pop(0))
            while pend:
                att_PV(*pend.pop(0))
            self.release(mB)
            if DBG_STAGE == "E":
                st = sb("dbgl", [128, 2048], BF16)
                self.dma(st[:], mixT[:, 0, 0:2048], st.r, rd=[mixT.r], wr=[st.r])
                dump(st[:], 2048, [st.r]); return False
            zv = sb("zv", [128, 18, 512], BF16); zx1 = sb("zx1", [128, 18, 512], BF16); zx2 = sb("zx2", [128, 18, 512], BF16)
            zs = [zv, zx1, zx2]
            mC2 = self.mark()
            wst = sb("wst", [128, 8, 512]); wbf = [sb("wbf", [128, 8, 512], BF16) for _ in range(2)]
            U_ = [sb("U", [128, NT + 4]) for _ in range(3)]; cv_ = [sb("cv", [128, NT]) for _ in range(2)]; cvb_ = [sb("cvb", [128, NT], BF16) for _ in range(2)]
            swT = sb("swT", [128, 12, 3]); sbT = sb("sbT", [128, 12])
            gst = [sb("gst", [128, 4, 512], BF16) for _ in range(2)]
            self.dma(swT[:], swT_d[l], swT.r, wr=[swT.r])
            self.dma(sbT[:], sbT_d[l], sbT.r, wr=[sbT.r])
            for U in U_:
                self.memset("vector", U[:], 0.0, wr=[U.r])
            segs = [(0, L, 1)] + ([] if last else [(L, LC, 3)])
            blocks_h = TB[:4] if last else TB
            def c2_T0(k):
                g, cc = divmod(k, 4)
                wb = wbf[g % 2]
                if cc == 0:
                    load_w_cols(wb, wst, w_in[l], 1536 + g * 512, 512)
                U = U_[k % 3]
                for bi, (t0, n, s) in enumerate(blocks_h):
                    p = self.psum()
                    for dc in range(8):
                        self.mm(p, p[:, 0:n], wb[:, dc, cc * 128:(cc + 1) * 128], hT[:, dc, t0:t0 + n], dc == 0, dc == 7, [wb.r, hT.r])
                    uo = t0 + (1 if s == 0 else 3)
                    self.cp("scalar" if bi % 2 else "vector", U[:, uo:uo + n], p[:, 0:n], [p.r], acc=[U.r])

            def c2_T1(k):
                U, cv = U_[k % 3], cv_[k % 2]
                for (s0, n, sh) in segs:
                    u0 = s0 + sh
                    self.act(cv[:, s0:s0 + n], U[:, u0:u0 + n], AF.Identity, [U.r, swT.r, sbT.r], acc=[cv.r], scale=swT[:, k, 1:2], bias=sbT[:, k:k + 1])

            def c2_T2(k):
                U, cv, cvb = U_[k % 3], cv_[k % 2], cvb_[k % 2]
                for (s0, n, sh) in segs:
                    u0 = s0 + sh
                    self.stt("vector", cv[:, s0:s0 + n], U[:, u0 - 1:u0 - 1 + n], swT[:, k, 0:1], cv[:, s0:s0 + n], ALU.mult, ALU.add, [U.r, swT.r, cv.r], acc=[cv.r])
                    self.stt("vector", cvb[:, s0:s0 + n], U[:, u0 + 1:u0 + 1 + n], swT[:, k, 2:3], cv[:, s0:s0 + n], ALU.mult, ALU.add, [U.r, swT.r, cv.r], acc=[cvb.r])

            def c2_T3(k):
                g, cc = divmod(k, 4)
                cvb = cvb_[k % 2]
                for t4 in range(0, ntile, 4):
                    nn = min(4, ntile - t4)
                    transpose4(lambda j: cvb[:, (t4 + j) * 128:(t4 + j + 1) * 128], nn, zs[g][:, t4:t4 + nn, cc * 128:(cc + 1) * 128], zs[g], [cvb.r],
                               "scalar" if (t4 // 4) % 2 else "vector")

            c2st = [c2_T0, c2_T1, c2_T2, c2_T3]
            for step in range(12 + 3):
                for kk, fn in enumerate(c2st):
                    i = step - kk
                    if 0 <= i < 12:
                        fn(i)
            for g in range(4):
                wb = wbf[g % 2]
                load_w_cols(wb, wst, w_in[l], 3072 + g * 512, 512)
                for bi, (t0, n, s) in enumerate(blocks_h):
                    gs = gst[bi % 2]
                    for cc in range(4):
                        p = self.psum()
                        for dc in range(8):
                            self.mm(p, p[:, 0:n], wb[:, dc, cc * 128:(cc + 1) * 128], hT[:, dc, t0:t0 + n], dc == 0, dc == 7, [wb.r, hT.r])
                        self.act(gs[:, cc, 0:n], p[:, 0:n], AF.Sigmoid, [p.r], wr=[gs.r] if cc == 0 else (), acc=() if cc == 0 else [gs.r])
                    self.dma(gscr[:, g * 4:(g + 1) * 4, t0:t0 + n], gs[:, :, 0:n], gs.r, rd=[gs.r], acc=[gscr.r])
            self.release(mC2)
            if DBG_STAGE == "C2":
                dump(zv[:, 0:4, :].rearrange("p a b -> p (a b)"), 2048, [zv.r]); return False
            skipB = sb("skipB", [128, 2, 512])
            self.dma(skipB[:], skip_d[l].partition_broadcast(128), skipB.r, wr=[skipB.r])
            Y = sb("Y", [128, 16, 2, 512], BF16)
            tc_ = [sb("tC", [128, 16, 128], BF16) for _ in range(2)]
            ts_ = [sb("tS", [128, 16, 128], BF16) for _ in range(2)]
            kf = [sb("kf", [128, 2, 512]) for _ in range(2)]
            ht = [sb("ht", [128, 512]) for _ in range(4)]
            hst = [sb("hst", [128, 4, 128], BF16) for _ in range(2)]
            streams = [(tabs["m"], 0)] + ([] if last else [(tabs["c"], 16)])
            for tb, tile0 in streams:
                nt = tb["nt"]
                for o in range(2):
                    zin = zs[o]
                    gate = zs[o + 1]
                    for fc in range(nt):
                        tC, tS, kk = tc_[fc % 2], ts_[fc % 2], kf[fc % 2]
                        self.dma(tC[:, 0:nt, :], tb["CF"][fc], tC.r, wr=[tC.r])
                        self.dma(tS[:, 0:nt, :], tb["SF"][fc], tS.r, wr=[tS.r])
                        self.dma(kk[:], tb["KF"][l][fc, :, :, o * 512:(o + 1) * 512], kk.r, rd=[tb["KF"][l].r], wr=[kk.r])
                        pr, pi = self.psum(), self.psum()
                        for st in range(nt):
                            self.mm(pr, pr[:, :], tC[:, st, :], zin[:, tile0 + st, :], st == 0, st == nt - 1, [tC.r, zin.r])
                        for st in range(nt):
                            self.mm(pi, pi[:, :], tS[:, st, :], zin[:, tile0 + st, :], st == 0, st == nt - 1, [tS.r, zin.r])
                        self.tt("vector", ht[0][:], pr[:, :], kk[:, 0, :], ALU.mult, [pr.r, kk.r], wr=[ht[0].r])
                        self.tt("vector", ht[1][:], pi[:, :], kk[:, 1, :], ALU.mult, [pi.r, kk.r], wr=[ht[1].r])
                        self.tt("gpsimd", Y[:, fc, 0, :], ht[0][:], ht[1][:], ALU.add, [ht[0].r, ht[1].r], acc=[Y.r])
                        self.tt("vector", ht[2][:], pi[:, :], kk[:, 0, :], ALU.mult, [pi.r, kk.r], wr=[ht[2].r])
                        self.tt("vector", ht[3][:], pr[:, :], kk[:, 1, :], ALU.mult, [pr.r, kk.r], wr=[ht[3].r])
                        self.tt("gpsimd", Y[:, fc, 1, :], ht[2][:], ht[3][:], ALU.subtract, [ht[2].r, ht[3].r], acc=[Y.r])
                    for tt_ in range(nt):
                        tC, tS = tc_[tt_ % 2], ts_[tt_ % 2]
                        self.dma(tC[:, 0:nt, :], tb["CI"][tt_], tC.r, wr=[tC.r])
                        self.dma(tS[:, 0:nt, :], tb["SI"][tt_], tS.r, wr=[tS.r])
                        p = self.psum()
                        for fc in range(nt):
                            self.mm(p, p[:, :], tC[:, fc, :], Y[:, fc, 0, :], fc == 0, False, [tC.r, Y.r])
                            self.mm(p, p[:, :], tS[:, fc, :], Y[:, fc, 1, :], False, fc == nt - 1, [tS.r, Y.r])
                        tl = tile0 + tt_
                        a0, a1 = ht[tt_ % 2], ht[2 + tt_ % 2]
                        self.tt("gpsimd", a0[:], zin[:, tl, :], skipB[:, o, :], ALU.mult, [zin.r, skipB.r], wr=[a0.r])
                        self.tt("vector", a1[:], p[:, :], a0[:], ALU.add, [p.r, a0.r], wr=[a1.r])
                        self.tt("vector", gate[:, tl, :], a1[:], gate[:, tl, :], ALU.mult, [a1.r, gate.r], acc=[gate.r])
                for t in range(tile0, tile0 + nt):
                    h_ = hst[t % 2]
                    p = self.psum()
                    for j in range(4):
                        self.mm_new(p, p[:, j * 128:(j + 1) * 128], zx2[:, t, j * 128:(j + 1) * 128], identb[:], True, True, [zx2.r, identb.r], j == 0)
                    self.cp("scalar", h_[:], p[:, :].rearrange("p (j c) -> p j c", j=4), [p.r], wr=[h_.r])
                    self.dma(mixT[:, 4:8, t * 128:(t + 1) * 128], h_[:], h_.r, rd=[h_.r], acc=[mixT.r])
            self.release(m0)
            if DBG_STAGE == "H":
                st = sb("dbgl", [128, 2048], BF16)
                self.dma(st[:], mixT[:, 4, 0:2048], st.r, rd=[mixT.r], wr=[st.r])
                dump(st[:], 2048, [st.r]); return False
            xT = sb("xT", [128, 8, NT])
            mM = self.mark()
            wa = sb("wa", [128, 4, D], BF16); wbb = sb("wbb", [128, 4, D], BF16); wo = sb("wo", [128, 8, D], BF16)
            wst4_ = [sb("wst4", [128, 4, D]) for _ in range(2)]
            mci = 0
            def m_load(dst, src, r0, mci):
                wst4 = wst4_[mci % 2]
                self.dma(wst4[:], src[r0:r0 + 512, :].rearrange("(a p) c -> p a c", p=128), wst4.r, wr=[wst4.r])
                ceng = ("gpsimd", "vector", "scalar", "gpsimd")[mci % 4]
                if r0 == 0:
                    self.cp(ceng, dst[:, 0:4, :], wst4[:], [wst4.r], wr=[dst.r])
                else:
                    self.cp(ceng, dst[:, 4:8, :], wst4[:], [wst4.r], acc=[dst.r])

            m_load(wa, wbra_d[l], 0, 0)
            m_load(wbb, wbrb_d[l], 0, 1)
            mixb = [sb("mixb", [128, 8, 512], BF16) for _ in range(2)]
            gb = [sb("gb", [128, 2, 512], BF16) for _ in range(4)]
            yT = sb("yT", [128, 8, 512], BF16)
            ta = [sb("ta", [128, 512]) for _ in range(2)]; tbm = [sb("tbm", [128, 512]) for _ in range(2)]
            for bi, (t0, n, s) in enumerate(blocks_upd):
                mb = mixb[bi % 2]
                self.dma(mb[:, :, 0:n], mixT[:, :, t0:t0 + n], mb.r, rd=[mixT.r], wr=[mb.r])
                for yc in range(8):
                    g_ = gb[yc % 4]
                    self.dma(g_[:, 0, 0:n], gscr[:, yc, t0:t0 + n], g_.r, rd=[gscr.r], wr=[g_.r])
                    self.dma(g_[:, 1, 0:n], gscr[:, 8 + yc, t0:t0 + n], g_.r, rd=[gscr.r], acc=[g_.r])
                    if bi == 0 and yc == 1:
                        m_load(wo, wout_d[l], 0, 2)
                    if bi == 0 and yc == 2:
                        m_load(wo, wout_d[l], 512, 3)
                    if bi == 0 and yc == 3:
                        self.dma(xT[:], xscr[:], xT.r, rd=[xscr.r], wr=[xT.r])
                    pa, pb = self.psum(), self.psum()
                    for ac in range(4):
                        self.mm(pa, pa[:, 0:n], wa[:, ac, yc * 128:(yc + 1) * 128], mb[:, ac, 0:n], ac == 0, ac == 3, [wa.r, mb.r])
                    for ac in range(4):
                        self.mm(pb, pb[:, 0:n], wbb[:, ac, yc * 128:(yc + 1) * 128], mb[:, 4 + ac, 0:n], ac == 0, ac == 3, [wbb.r, mb.r])
                    a_, b_ = ta[yc % 2], tbm[yc % 2]
                    self.tt("vector", a_[:, 0:n], pa[:, 0:n], g_[:, 0, 0:n], ALU.mult, [pa.r, g_.r], wr=[a_.r])
                    self.tt("vector", b_[:, 0:n], pb[:, 0:n], g_[:, 1, 0:n], ALU.mult, [pb.r, g_.r], wr=[b_.r])
                    self.tt("gpsimd", yT[:, yc, 0:n], a_[:, 0:n], b_[:, 0:n], ALU.add, [a_.r, b_.r], wr=[yT.r] if yc == 0 else (), acc=() if yc == 0 else [yT.r])
                for dc in range(8):
                    p = self.psum()
                    for yc in range(8):
                        self.mm(p, p[:, 0:n], wo[:, yc, dc * 128:(dc + 1) * 128], yT[:, yc, 0:n], yc == 0, yc == 7, [wo.r, yT.r])
                    self.stt("vector", xT[:, dc, t0:t0 + n], p[:, 0:n], scl[:, s, 2, dc:dc + 1], xT[:, dc, t0:t0 + n], ALU.mult, ALU.add, [p.r, scl.r, xT.r], acc=[xT.r])
            self.release(mM)
            if DBG_STAGE == "M":
                dump(xT[:, 0, 0:2304], 2304, [xT.r]); return False
            h2T = sb("h2T", [128, 8, NT], BF16)
            cwT = sb("cwT", [32, NT], BF16)
            mN = self.mark()
            h2f_ = [sb("h2f", [128, 8, 512]) for _ in range(2)]
            wr_ = sb("wr", [128, 8, 36]); brB = sb("brB", [128, 36])
            lg = sb("lg", [128, 18, 36]); sm = sb("sm", [128, 12, 18]); ohg = sb("ohg", [128, 18, 4]); eg = sb("eg", [128, 18, 4])
            em = sb("em", [128, 18, 32]); em2 = sb("em2", [128, 18, 32]); oh1 = sb("oh1", [128, 18, 32]); oh2 = sb("oh2", [128, 18, 32])
            cwb = sb("cwb", [128, 18, 32], BF16)
            self.dma(wr_[:], wgr_d[l].rearrange("(dc p) c -> p dc c", p=128), wr_.r, wr=[wr_.r])
            self.dma(brB[:], bgr_d[l].partition_broadcast(128), brB.r, wr=[brB.r])

            NTL = 16 if last else 18
            pR = [self.ps[6], self.ps[7]]

            def route(t0, n, s, h2f):
                for tt_ in range(n // 128):
                    tl = t0 // 128 + tt_
                    pr = pR[tl // 9]
                    co = (tl % 9) * 36
                    c0 = tt_ * 128
                    for dc in range(8):
                        self.mm_new(pr, pr[:, co:co + 36], h2f[:, dc, c0:c0 + 128], wr_[:, dc, :], dc == 0, dc == 7, [h2f.r, wr_.r], (tl % 9 == 0) and dc == 0)

            def route_all():
                nt_ = NTL
                B3 = lambda ap, k: ap.unsqueeze(2).to_broadcast([128, nt_, k])
                for hb in range(2):
                    na = min(9, nt_ - hb * 9)
                    self.tt("vector", lg[:, hb * 9:hb * 9 + na, :], pR[hb][:, 0:na * 36].rearrange("p (t c) -> p t c", c=36),
                            brB[:].unsqueeze(1).to_broadcast([128, na, 36]), ALU.add, [pR[hb].r, brB.r], wr=[lg.r] if hb == 0 else (), acc=() if hb == 0 else [lg.r])
                LG, LE = lg[:, 0:nt_, 0:4], lg[:, 0:nt_, 4:36]
                S = lambda k: sm[:, k, 0:nt_]
                self.red(S(0), LG, "max", [lg.r], wr=[sm.r])
                self.tt("vector", ohg[:, 0:nt_, :], LG, B3(S(0), 4), ALU.is_equal, [lg.r, sm.r], wr=[ohg.r])
                self.tt("vector", eg[:, 0:nt_, :], LG, B3(S(0), 4), ALU.subtract, [lg.r, sm.r], wr=[eg.r])
                self.act(eg[:, 0:nt_, :], eg[:, 0:nt_, :], AF.Exp, [eg.r], wr=[eg.r])
                self.red(S(2), eg[:, 0:nt_, :], "sum", [eg.r], acc=[sm.r])
                self.recip(S(3), S(2), [sm.r], acc=[sm.r])
                self.ts("vector", ohg[:, 0:nt_, :], ohg[:, 0:nt_, :], 1.0, 1e9, ALU.subtract, ALU.mult, [ohg.r], wr=[ohg.r])
                self.tt("vector", em[:, 0:nt_, :].rearrange("p t (g e) -> p t g e", g=4), LE.rearrange("p t (g e) -> p t g e", g=4),
                        ohg[:, 0:nt_, :].unsqueeze(3).to_broadcast([128, nt_, 4, 8]), ALU.add, [lg.r, ohg.r], wr=[em.r])
                self.red(S(4), em[:, 0:nt_, :], "max", [em.r], acc=[sm.r])
                self.tt("vector", oh1[:, 0:nt_, :], em[:, 0:nt_, :], B3(S(4), 32), ALU.is_equal, [em.r, sm.r], wr=[oh1.r])
                self.stt("vector", em2[:, 0:nt_, :], oh1[:, 0:nt_, :], -1e9, em[:, 0:nt_, :], ALU.mult, ALU.add, [oh1.r, em.r], wr=[em2.r])
                self.red(S(5), em2[:, 0:nt_, :], "max", [em2.r], acc=[sm.r])
                self.tt("vector", oh2[:, 0:nt_, :], em2[:, 0:nt_, :], B3(S(5), 32), ALU.is_equal, [em2.r, sm.r], wr=[oh2.r])
                self.tt("vector", S(6), S(5), S(4), ALU.subtract, [sm.r], acc=[sm.r])
                self.act(S(7), S(6), AF.Exp, [sm.r], acc=[sm.r])
                self.ts("vector", S(8), S(7), 1.0, None, ALU.add, None, [sm.r], acc=[sm.r])
                self.recip(S(9), S(8), [sm.r], acc=[sm.r])
                self.tt("vector", S(10), S(7), S(9), ALU.mult, [sm.r], acc=[sm.r])
                self.tt("vector", S(9), S(9), S(3), ALU.mult, [sm.r], acc=[sm.r])
                self.tt("vector", S(10), S(10), S(3), ALU.mult, [sm.r], acc=[sm.r])
                self.tt("vector", oh1[:, 0:nt_, :], oh1[:, 0:nt_, :], B3(S(9), 32), ALU.mult, [oh1.r, sm.r], wr=[oh1.r])
                self.tt("vector", oh2[:, 0:nt_, :], oh2[:, 0:nt_, :], B3(S(10), 32), ALU.mult, [oh2.r, sm.r], wr=[oh2.r])
                self.tt("vector", cwb[:, 0:nt_, :], oh1[:, 0:nt_, :], oh2[:, 0:nt_, :], ALU.add, [oh1.r, oh2.r], wr=[cwb.r])
                for t4 in range(0, nt_, 4):
                    nn = min(4, nt_ - t4)
                    p2 = self.psum()
                    for j in range(nn):
                        self.mm_new(p2, p2[0:32, j * 128:(j + 1) * 128], cwb[:, t4 + j, :], identb[:], True, True, [cwb.r, identb.r], j == 0)
                    self.cp("scalar" if (t4 // 4) % 2 else "vector", cwT[:, t4 * 128:(t4 + nn) * 128], p2[0:32, 0:nn * 128], [p2.r], acc=[cwT.r])

            norm_blocks(xT, h2T, 3, 4, blocks_upd, hook=route, f32buf=h2f_)
            route_all()
            self.release(mN)
            if DBG_STAGE == "N":
                dump(cwT[:, 0:2304], 2304, [cwT.r]); return False
            sel = sb("sel", [32, 32 * 128], BF16)
            self.dma(sel[:], sel_d, sel.r, wr=[sel.r])
            stg = [sb("wstg", [128, 1024]) for _ in range(2)]
            wb1 = [[sb("wb1", [128, 8, 256], BF16) for _ in range(2)] for _ in range(2)]
            wb3 = [[sb("wb3", [128, 8, 256], BF16) for _ in range(2)] for _ in range(2)]
            wb2 = [[sb("wb2", [128, 2, D], BF16) for _ in range(2)] for _ in range(2)]
            sa = [sb("sa", [128, 512]) for _ in range(4)]
            cws = [[sb("cws", [128, 512], BF16) for _ in range(2)] for _ in range(2)]
            hid = [sb("hid", [128, 4, 512], BF16) for _ in range(2)]
            cnt = 0
            ycnt = 0
            kst_ = 0
            for ep in range(16):
                pbuf = ep % 2
                for j in range(2):
                    e = 2 * ep + j
                    b1_, b3_, b2_ = wb1[pbuf][j], wb3[pbuf][j], wb2[pbuf][j]
                    for (dst, src) in ((b1_, mw1_d[l, e]), (b3_, mw3_d[l, e])):
                        v = src.rearrange("(dc p) f -> p dc f", p=128)
                        for hh in range(2):
                            sg = stg[kst_ % 2]
                            kst_ += 1
                            self.dma(sg[:].rearrange("p (a f) -> p a f", a=4), v[:, hh * 4:(hh + 1) * 4, :], sg.r, wr=[sg.r])
                            self.cp("gpsimd", dst[:, hh * 4:(hh + 1) * 4, :], sg[:].rearrange("p (a f) -> p a f", a=4), [sg.r],
                                    wr=[dst.r] if hh == 0 else (), acc=() if hh == 0 else [dst.r])
                    v = mw2_d[l, e].rearrange("(fc p) d -> p fc d", p=128)
                    for hh in range(2):
                        sg = stg[kst_ % 2]
                        kst_ += 1
                        self.dma(sg[:], v[:, hh, :], sg.r, wr=[sg.r])
                        self.cp("gpsimd", b2_[:, hh, :], sg[:], [sg.r], wr=[b2_.r] if hh == 0 else (), acc=() if hh == 0 else [b2_.r])
                for (t0, n, s) in blocks_upd:
                    hd_ = hid[cnt % 2]
                    cw_ = cws[cnt % 2]
                    cnt += 1
                    for j in range(2):
                        e = 2 * ep + j
                        pc = self.ps[ycnt % 8]
                        ycnt += 1
                        self.mm(pc, pc[:, 0:n], sel[:, e * 128:(e + 1) * 128], cwT[:, t0:t0 + n], True, True, [sel.r, cwT.r])
                        self.cp("scalar", cw_[j][:, 0:n], pc[:, 0:n], [pc.r], wr=[cw_[j].r])
                    for j in range(2):
                        b1_, b3_ = wb1[pbuf][j], wb3[pbuf][j]
                        for fc in range(2):
                            k = j * 2 + fc
                            pa, pb = self.ps[ycnt % 8], self.ps[(ycnt + 1) % 8]
                            ycnt += 2
                            for dc in range(8):
                                self.mm(pa, pa[:, 0:n], b1_[:, dc, fc * 128:(fc + 1) * 128], h2T[:, dc, t0:t0 + n], dc == 0, dc == 7, [b1_.r, h2T.r])
                            for dc in range(8):
                                self.mm(pb, pb[:, 0:n], b3_[:, dc, fc * 128:(fc + 1) * 128], h2T[:, dc, t0:t0 + n], dc == 0, dc == 7, [b3_.r, h2T.r])
                            s_ = sa[k]
                            self.act(s_[:, 0:n], pa[:, 0:n], AF.Silu, [pa.r], wr=[s_.r])
                            self.tt("vector", s_[:, 0:n], pb[:, 0:n], s_[:, 0:n], ALU.mult, [pb.r, s_.r], wr=[s_.r])
                            self.tt("vector", hd_[:, k, 0:n], cw_[j][:, 0:n], s_[:, 0:n], ALU.mult, [cw_[j].r, s_.r], wr=[hd_.r] if k == 0 else (), acc=() if k == 0 else [hd_.r])
                    for dc in range(8):
                        p = self.ps[ycnt % 8]
                        ycnt += 1
                        for k in range(4):
                            j, fc = divmod(k, 2)
                            self.mm(p, p[:, 0:n], wb2[pbuf][j][:, fc, dc * 128:(dc + 1) * 128], hd_[:, k, 0:n], k == 0, k == 3, [wb2[pbuf][j].r, hd_.r])
                        self.stt("vector", xT[:, dc, t0:t0 + n], p[:, 0:n], scl[:, s, 5, dc:dc + 1], xT[:, dc, t0:t0 + n], ALU.mult, ALU.add, [p.r, scl.r, xT.r], acc=[xT.r])
            if DBG_STAGE == "X":
                dump(xT[:, 0, 0:2304], 2304, [xT.r]); return False
            if not last:
                self.dma(xscr[:], xT[:], xT.r, rd=[xT.r], wr=[xscr.r])
            else:
                self.release(mN)
                ost = [sb("ost", [128, D]) for _ in range(2)]
                for t in range(16):
                    o_ = ost[t % 2]
                    for half in range(2):
                        p = self.psum()
                        for j in range(4):
                            dc = half * 4 + j
                            self.tr32(p, p[:, j * 128:(j + 1) * 128], xT[:, dc, t * 128:(t + 1) * 128], identf[:], [xT.r, identf.r], j == 0)
                        self.cp("scalar" if half else "vector", o_[:, half * 512:(half + 1) * 512], p[:, :], [p.r], wr=[o_.r] if half == 0 else (), acc=() if half == 0 else [o_.r])
                    self.dma(out_d[b, t * 128:(t + 1) * 128, :], o_[:], o_.r, rd=[o_.r], acc=[out_r])
            self.release(m0)
            return True

        nb = int(os.environ.get("MK_NB", "2"))
        nl = int(os.environ.get("MK_NL", str(DEPTH)))
        ok = True
        for b in range(nb):
            phase_load(b)
            for l in range(nl):
                if b == 0:
                    phase_emask(l)
                    phase_filter(l, tabs["m"])
                    if l != DEPTH - 1:
                        phase_filter(l, tabs["c"])
                ok = layer_body(b, l, l == DEPTH - 1)
                if not ok:
                    break
            if not ok:
                break
        self.P.barrier()
        if not dbg_done[0]:
            st = sb("dbgz", [128, 64])
            self.memset("vector", st[:], 0.0, wr=[st.r])
            self.dma(dbg_d[:, 0:64], st[:], st.r, rd=[st.r], wr=[Res("dbgo")])
            self.P.barrier()
        self.P.build()
        return self.nc


_CACHE = {}


def _host_consts():
    if "c" in _CACHE:
        return _CACHE["c"]
    c = {}
    c["ident"] = np.eye(128, dtype=np.float32)
    c["rope_cos"], c["rope_sin"] = _rope_tables()
    sel = np.zeros((32, 32, 128), np.float32)
    for e in range(32):
        sel[e, e, :] = 1.0
    c["sel"] = sel.reshape(32, 32 * 128).astype(ml_dtypes.bfloat16)
    for tag, Ls in (("m", L), ("c", LC)):
        zT, dec = _filter_consts(Ls)
        c[f"zT_{tag}"] = zT
        c[f"dec_{tag}"] = dec
        t = _dft_tables(Ls)
        for k in ("CF", "SF", "CI", "SI"):
            c[f"{k}_{tag}"] = t[k]
    _CACHE["c"] = c
    return c


def kernel(x, c, ctx, c_ctx, ada_w, ada_b, norm_mix, norm_ffn, w_in, q_norm, k_norm, rpb,
           short_w, short_b, flt_w1, flt_b1, flt_w2, flt_b2, flt_w3, hy_skip, w_br_a, w_br_b, w_out,
           w_group, b_group, w_router, b_router, moe_w1, moe_w3, moe_w2):
    f = lambda a: np.ascontiguousarray(np.asarray(a, dtype=np.float32))
    x, c, ctx, c_ctx = f(x), f(c), f(ctx), f(c_ctx)
    shared = dict(_host_consts())
    shared["ada_w"] = f(ada_w)
    shared["ada_bT"] = f(np.asarray(ada_b).reshape(DEPTH, 48, 128).transpose(0, 2, 1))
    shared["norm_mixT"] = f(np.asarray(norm_mix).reshape(DEPTH, 8, 128).transpose(0, 2, 1))
    shared["norm_ffnT"] = f(np.asarray(norm_ffn).reshape(DEPTH, 8, 128).transpose(0, 2, 1))
    shared["w_in"] = f(w_in)
    shared["q_norm"] = f(q_norm)
    shared["k_norm"] = f(k_norm)
    shared["biasT"] = _bias_tables(f(rpb)).reshape(DEPTH, 5, 128, 8 * 5 * 128)
    shared["short_wT"] = f(np.asarray(short_w).reshape(DEPTH, 3, 12, 128).transpose(0, 3, 2, 1))
    shared["short_bT"] = f(np.asarray(short_b).reshape(DEPTH, 12, 128).transpose(0, 2, 1))
    shared["flt_w1"] = f(flt_w1)
    shared["flt_b1"] = f(np.asarray(flt_b1).reshape(DEPTH, 64, 1))
    shared["flt_w2"] = f(flt_w2)
    shared["flt_b2"] = f(np.asarray(flt_b2).reshape(DEPTH, 64, 1))
    shared["flt_w3"] = f(flt_w3)
    shared["hy_skip"] = f(hy_skip)
    shared["w_br_a"] = f(w_br_a)
    shared["w_br_b"] = f(w_br_b)
    shared["w_out"] = f(w_out)
    shared["w_gr"] = f(np.concatenate([np.asarray(w_group), np.asarray(w_router)], axis=-1))
    shared["b_gr"] = f(np.concatenate([np.asarray(b_group), np.asarray(b_router)], axis=-1))
    shared["moe_w1"] = f(np.asarray(moe_w1).reshape(DEPTH, 32, D, 256))
    shared["moe_w3"] = f(np.asarray(moe_w3).reshape(DEPTH, 32, D, 256))
    shared["moe_w2"] = f(np.asarray(moe_w2).reshape(DEPTH, 32, 256, D))
    if "nc" not in _CACHE:
        _CACHE["nc"] = KB().build()
    nc = _CACHE["nc"]
    in_maps = []
    for i in range(NCORES):
        m = dict(shared)
        m["x"] = x[2 * i:2 * i + 2]
        m["ctx"] = ctx[2 * i:2 * i + 2]
        cT = np.empty((2, 128, 8, 2), np.float32)
        for bb in range(2):
            cT[bb, :, :, 0] = c[2 * i + bb].reshape(8, 128).T
            cT[bb, :, :, 1] = c_ctx.reshape(8, 128).T
        m["cT"] = cT
        in_maps.append(m)
    res = run_bass_kernel_spmd(nc, in_maps, core_ids=list(range(NCORES)))
    _CACHE["last"] = res
    out = np.concatenate([r["out"] for r in res.results], axis=0)
    return out.astype(np.float32)
```
